# Optimizing a Trainium2 kernel written in Bass

```python
import jax, jax.numpy as jnp
from jax import lax
import numpy as np

D_MODEL = 1024
BATCH = 16
SEQ = 4096
DEPTH = 1

D_MIX = D_MODEL
GLA_HEADS = 4
GLA_DK = 64
GLA_DV = 128
GLA_RANK = 16
GLA_GATE_NORM = 16.0
GLA_CHUNK = 64
SB_HEADS = 8
SB_DH = 64
SB_BLOCK = 128
D_FF = 2816
EPS = 1e-6

GLA_QK = GLA_HEADS * GLA_DK
GLA_V = GLA_HEADS * GLA_DV
SB_W = SB_HEADS * SB_DH
PROJ_SIZES = (GLA_QK, GLA_QK, GLA_V, GLA_V, GLA_RANK, SB_W, SB_W, SB_W)
PROJ_SPLITS = tuple(int(s) for s in np.cumsum(PROJ_SIZES)[:-1])
D_IN = int(sum(PROJ_SIZES))

kernel_name = "hymba_style_gla_stickbreaking_macaron"


def rmsnorm(x, g):
    xf = x.astype(jnp.float32)
    y = xf * lax.rsqrt(jnp.mean(xf * xf, axis=-1, keepdims=True) + EPS)
    return (y * g.astype(jnp.float32)).astype(x.dtype)


def swiglu(h, w_gate, w_up, w_down):
    return (jax.nn.silu(h @ w_gate) * (h @ w_up)) @ w_down


def gla_chunked(q, k, v, log_a):
    B, T, H, DK = q.shape
    DV = v.shape[-1]
    C = GLA_CHUNK
    N = T // C

    def chunks(t):
        return t.reshape(B, N, C, H, t.shape[-1]).transpose(0, 3, 1, 2, 4)

    q, k, v, log_a = chunks(q * (GLA_DK ** -0.5)), chunks(k), chunks(v), chunks(log_a)
    b = jnp.cumsum(log_a, axis=3)
    b_last = b[:, :, :, -1:, :]
    q_dec = q * jnp.exp(b)
    k_inv = k * jnp.exp(-b)
    k_end = k * jnp.exp(b_last - b)
    causal = jnp.tril(jnp.ones((C, C), dtype=bool))
    scores = jnp.where(causal, jnp.einsum('bhnid,bhnjd->bhnij', q_dec, k_inv), 0.0)
    o_intra = jnp.einsum('bhnij,bhnjv->bhniv', scores, v)

    def step(state, xs):
        k_n, v_n, decay_n = xs
        new = decay_n[..., None] * state + jnp.einsum('bhcd,bhcv->bhdv', k_n, v_n)
        return new, state

    xs = (jnp.moveaxis(k_end, 2, 0), jnp.moveaxis(v, 2, 0),
          jnp.moveaxis(jnp.exp(b_last[:, :, :, 0, :]), 2, 0))
    _, states = lax.scan(step, jnp.zeros((B, H, DK, DV), jnp.float32), xs)
    states = jnp.moveaxis(states, 0, 2)
    o_inter = jnp.einsum('bhnid,bhndv->bhniv', q_dec, states)
    o = o_intra + o_inter
    return o.transpose(0, 2, 3, 1, 4).reshape(B, T, H, DV)


def stick_breaking(q, k, v):
    B, T, H, D = q.shape
    NB = T // SB_BLOCK
    scale = D ** -0.5
    q = q.transpose(0, 2, 1, 3)
    k = k.transpose(0, 2, 1, 3)
    v = v.transpose(0, 2, 1, 3)
    q_blocks = q.reshape(B, H, NB, SB_BLOCK, D).transpose(2, 0, 1, 3, 4)
    key_pos = jnp.arange(T)

    def block(args):
        qb, i = args
        z = jnp.einsum('bhqd,bhkd->bhqk', qb, k) * scale
        t_pos = i * SB_BLOCK + jnp.arange(SB_BLOCK)
        mask = key_pos[None, :] < t_pos[:, None]
        log_keep = jnp.where(mask, jax.nn.log_sigmoid(-z), 0.0)
        between = lax.cumsum(log_keep, axis=3, reverse=True) - log_keep
        w = jnp.where(mask, jnp.exp(jax.nn.log_sigmoid(z) + between), 0.0)
        return jnp.einsum('bhqk,bhkd->bhqd', w, v)

    out = lax.map(block, (q_blocks, jnp.arange(NB)))
    return out.transpose(1, 0, 3, 2, 4).reshape(B, T, H, D)


def setup_inputs(seed: int = 0) -> dict:
    key = jax.random.key(seed)
    ks = jax.random.split(key, 20)
    f32 = jnp.float32

    def w(k, shape, fan_in):
        return jax.random.normal(k, shape, f32) * (fan_in ** -0.5)

    def gain(k, n):
        return 1.0 + 0.02 * jax.random.normal(k, (DEPTH, n), f32)

    return {
        "x": jax.random.normal(ks[0], (BATCH, SEQ, D_MODEL), f32),
        "ffn1_norm": gain(ks[1], D_MODEL),
        "ffn1_w_gate": w(ks[2], (DEPTH, D_MODEL, D_FF), D_MODEL),
        "ffn1_w_up": w(ks[3], (DEPTH, D_MODEL, D_FF), D_MODEL),
        "ffn1_w_down": w(ks[4], (DEPTH, D_FF, D_MODEL), D_FF),
        "mix_norm": gain(ks[5], D_MODEL),
        "w_in": w(ks[6], (DEPTH, D_MODEL, D_IN), D_MODEL),
        "w_gk_up": w(ks[7], (DEPTH, GLA_RANK, GLA_QK), GLA_RANK),
        "b_gk": 0.1 * jax.random.normal(ks[8], (DEPTH, GLA_QK), f32),
        "gla_out_norm": gain(ks[9], GLA_DV),
        "sb_q_norm": gain(ks[10], SB_DH),
        "sb_k_norm": gain(ks[11], SB_DH),
        "w_out": w(ks[12], (DEPTH, D_MIX, D_MODEL), D_MIX),
        "ffn2_norm": gain(ks[13], D_MODEL),
        "ffn2_w_gate": w(ks[14], (DEPTH, D_MODEL, D_FF), D_MODEL),
        "ffn2_w_up": w(ks[15], (DEPTH, D_MODEL, D_FF), D_MODEL),
        "ffn2_w_down": w(ks[16], (DEPTH, D_FF, D_MODEL), D_FF),
    }


def reference(x, ffn1_norm, ffn1_w_gate, ffn1_w_up, ffn1_w_down, mix_norm, w_in,
              w_gk_up, b_gk, gla_out_norm, sb_q_norm, sb_k_norm, w_out,
              ffn2_norm, ffn2_w_gate, ffn2_w_up, ffn2_w_down):
    B, T, _ = x.shape
    f32 = jnp.float32
    for l in range(DEPTH):
        x = x + 0.5 * swiglu(rmsnorm(x, ffn1_norm[l]), ffn1_w_gate[l], ffn1_w_up[l], ffn1_w_down[l])

        h = rmsnorm(x, mix_norm[l])
        p = h @ w_in[l]
        g_q, g_k, g_v, g_gate, g_lr, s_q, s_k, s_v = jnp.split(p, PROJ_SPLITS, axis=-1)

        log_a = jax.nn.log_sigmoid((g_lr @ w_gk_up[l] + b_gk[l]).astype(f32)) / GLA_GATE_NORM
        o_gla = gla_chunked(g_q.astype(f32).reshape(B, T, GLA_HEADS, GLA_DK),
                            g_k.astype(f32).reshape(B, T, GLA_HEADS, GLA_DK),
                            g_v.astype(f32).reshape(B, T, GLA_HEADS, GLA_DV),
                            log_a.reshape(B, T, GLA_HEADS, GLA_DK))
        o_gla = rmsnorm(o_gla, gla_out_norm[l]) * jax.nn.silu(
            g_gate.astype(f32).reshape(B, T, GLA_HEADS, GLA_DV))
        o_gla = o_gla.reshape(B, T, GLA_V)

        q_sb = rmsnorm(s_q.astype(f32).reshape(B, T, SB_HEADS, SB_DH), sb_q_norm[l])
        k_sb = rmsnorm(s_k.astype(f32).reshape(B, T, SB_HEADS, SB_DH), sb_k_norm[l])
        o_sb = stick_breaking(q_sb, k_sb, s_v.astype(f32).reshape(B, T, SB_HEADS, SB_DH))
        o_sb = o_sb.reshape(B, T, SB_W)

        mixed = jnp.concatenate([o_gla, o_sb], axis=-1).astype(x.dtype)
        x = x + mixed @ w_out[l]

        x = x + 0.5 * swiglu(rmsnorm(x, ffn2_norm[l]), ffn2_w_gate[l], ffn2_w_up[l], ffn2_w_down[l])
    return x
```

```python
import numpy as np
import concourse.bass as bass
import concourse.mybir as mybir
from concourse.bass_utils import run_bass_kernel_spmd

F32 = mybir.dt.float32
BF16 = mybir.dt.bfloat16
AF = mybir.ActivationFunctionType
ALU = mybir.AluOpType

NCORES = 8
D = 1024
DFF = 2816
NTOK = 8192
SEQ = 4096
TT = 512
KC = D // 128
FC = DFF // 128
DIN = 3088
EPS = 1e-6

SEM_CAP = 30000
import os
SBV_MODE = int(os.environ.get('SBV_MODE', '2'))
SBASE = 16512
SLIMIT = 229376


class Op:
    __slots__ = ("eng", "fn", "deps", "dma", "semkey", "need_inc", "inc", "waits", "idx", "bg")


class Prog:
    def __init__(self, nc):
        self.nc = nc
        self.ops = []
        self.last_w = {}
        self.readers = {}
        self.last_on_eng = {}
        self.last_dma_key = {}
        self.pending_bar = {}

    def op(self, eng, fn, reads=(), writes=(), dma=False, semkey=None, bg=False):
        o = Op()
        o.eng, o.fn, o.dma, o.semkey, o.bg = eng, fn, dma, semkey, bg
        o.idx = len(self.ops)
        o.need_inc = False
        o.inc = None
        o.waits = None
        deps = {}
        for k in reads:
            w = self.last_w.get(k)
            if w is not None:
                deps[w] = True
            if isinstance(k, tuple) and k[0] == "ps":
                for r in self.readers.get(k, ()):
                    if self.ops[r].eng != eng and r not in deps:
                        deps[r] = False
        for k in writes:
            w = self.last_w.get(k)
            if w is not None and w not in deps:
                deps[w] = False
            for r in self.readers.get(k, ()):
                if r not in deps:
                    deps[r] = False
        bar = self.pending_bar.pop(eng, None)
        if bar:
            for b in bar:
                deps[b] = True
        for k in reads:
            self.readers.setdefault(k, []).append(o.idx)
        for k in writes:
            self.last_w[k] = o.idx
            self.readers[k] = []
        o.deps = deps
        self.ops.append(o)
        self.last_on_eng[eng if not dma else ("dmaq", eng)] = o.idx
        if dma:
            if semkey is None:
                raise ValueError("dma needs semkey")
            o.semkey = semkey = "%s_%s" % (semkey, eng)
            if not bg:
                self.last_dma_key[semkey] = o.idx
        return o

    def barrier(self):
        lasts = [v for k, v in self.last_on_eng.items() if not isinstance(k, tuple)]
        lasts += list(self.last_dma_key.values())
        for e in ("pe", "act", "dve", "pool", "sp"):
            self.pending_bar[e] = list(lasts)

    def emit(self, stack):
        nc = self.nc
        ops = self.ops
        for o in ops:
            for d, raw in o.deps.items():
                od = ops[d]
                if od.dma:
                    continue
                if od.eng == o.eng and not o.dma:
                    if o.eng == "pe":
                        continue
                od.need_inc = True
        sems = {}

        def get_sem(name):
            if name not in sems:
                sems[name] = stack.enter_context(nc.semaphore(name))
            return sems[name]

        eng_cnt = {}
        dma_cnt = {}
        waited = {}
        boundary = {}
        dma_sem_eng = {}
        for o in ops:
            waits = []
            if o.dma:
                bv = boundary.get("d_%s" % (o.semkey,), 0)
                if bv > 0:
                    waits.append(("d_%s" % (o.semkey,), bv))
            for d, raw in o.deps.items():
                od = ops[d]
                if od.dma:
                    waits.append((od.inc[0], dma_cnt[od.semkey][1]))
                    continue
                if od.eng == o.eng and not o.dma:
                    if o.eng == "pe":
                        continue
                waits.append(od.inc)
            best = {}
            for s, v in waits:
                if v > best.get(s, 0):
                    best[s] = v
                if s.startswith("d_") and v > boundary.get(s, 0):
                    boundary[s] = v
            wl = []
            for s, v in best.items():
                key = (o.eng, s)
                if waited.get(key, 0) >= v:
                    continue
                waited[key] = v
                wl.append((s, v))
            o.waits = wl
            if o.dma:
                ep, cnt = dma_cnt.get(o.semkey, (None, 0))
                if ep is None:
                    ep = "d_%s" % (o.semkey,)
                    get_sem(ep)
                    cnt = 0
                assert cnt + 16 <= 60000
                cnt += 16
                dma_cnt[o.semkey] = (ep, cnt)
                dma_sem_eng[ep] = o.eng
                o.inc = (ep, cnt)
            elif o.need_inc:
                ep, cnt = eng_cnt.get(o.eng, (None, 0))
                if ep is None or cnt + 1 > SEM_CAP:
                    n = sum(1 for k in sems if k.startswith("e_%s_" % o.eng))
                    ep = "e_%s_%d" % (o.eng, n)
                    get_sem(ep)
                    cnt = 0
                cnt += 1
                eng_cnt[o.eng] = (ep, cnt)
                o.inc = (ep, cnt)
        final_waits = {}
        for (ep, cnt) in dma_cnt.values():
            final_waits.setdefault(dma_sem_eng[ep], []).append((ep, cnt))
        by_eng = {e: [] for e in ("pe", "act", "dve", "pool", "sp")}
        for o in ops:
            by_eng[o.eng].append(o)

        def run(engname, eng):
            for o in by_eng[engname]:
                for s, v in o.waits:
                    eng.wait_ge(sems[s], v)
                ins = o.fn(eng)
                if o.inc is not None:
                    ins.then_inc(sems[o.inc[0]], 16 if o.dma else 1)
            for s, v in final_waits.get(engname, ()):
                eng.wait_ge(sems[s], v)

        with nc.Block() as block:
            @block.tensor
            def _(e):
                run("pe", e)

            @block.scalar
            def _(e):
                run("act", e)

            @block.vector
            def _(e):
                run("dve", e)

            @block.gpsimd
            def _(e):
                run("pool", e)

            @block.sync
            def _(e):
                run("sp", e)


class SB:
    def __init__(self, nc):
        self.nc = nc
        self.n = 0

    def at(self, name, shape, dtype, off):
        self.n += 1
        return self.nc.alloc_sbuf_tensor_at("%s_%d" % (name, self.n), list(shape), dtype, offset=off)


def nbytes(shape, dtype):
    n = 1
    for s in shape[1:]:
        n *= s
    return n * (4 if dtype == F32 else 2)


class Arena:
    def __init__(self, sb, base, limit):
        self.sb, self.off, self.limit = sb, base, limit

    def alloc(self, name, shape, dtype):
        sz = (nbytes(shape, dtype) + 31) // 32 * 32
        t = self.sb.at(name, shape, dtype, self.off)
        self.off += sz
        if self.off > self.limit:
            raise RuntimeError("SBUF arena overflow at %s: %d > %d" % (name, self.off, self.limit))
        return t


def mm(p, ps, ps_keys, lhsT, rhs, rkeys, start, stop):
    p.op("pe", lambda e: e.matmul(ps, lhsT, rhs, start=start, stop=stop, skip_group_check=True), reads=rkeys,
         writes=ps_keys)


def rmsnorm_tile(p, xt, xkey, g_ap, hT, hkey, sq, sqkeys, ones_bf, ps_sum, ps_key, rstd, rstd_key, nelem, ckey="consts", sq_eng="pool"):
    for c in range(KC):
        s = sq[c % 2]
        sk = sqkeys[c % 2]
        if sq_eng == "act":
            p.op("act", lambda e, s=s, c=c: e.activation(s[:, :], xt[:, c, :], AF.Square), reads=[xkey], writes=[sk])
        else:
            p.op("pool", lambda e, s=s, c=c: e.tensor_tensor(s[:, :], xt[:, c, :], xt[:, c, :], ALU.mult),
                 reads=[xkey], writes=[sk])
        mm(p, ps_sum[:, :], [ps_key], ones_bf, s[:, :], [sk, ckey], c == 0, c == KC - 1)
    p.op("act", lambda e: e.activation(rstd[:, :], ps_sum[:, :], AF.Ln, bias=EPS, scale=1.0 / nelem),
         reads=[ps_key], writes=[rstd_key])
    p.op("act", lambda e: e.activation(rstd[:, :], rstd[:, :], AF.Exp, scale=-0.5),
         reads=[rstd_key], writes=[rstd_key])
    for c in range(KC):
        p.op("dve", lambda e, c=c: e.scalar_tensor_tensor(hT[:, c, :], xt[:, c, :], g_ap[:, c:c + 1], rstd[:, :],
                                                            ALU.mult, ALU.mult),
             reads=[xkey, rstd_key, ckey], writes=[hkey])


class Views:
    def __init__(self, ar, name, dtype, *shapes):
        off = ar.off
        self.v = [ar.sb.at(name, shp, dtype, off) for shp in shapes]
        sz = (nbytes(shapes[0], dtype) + 31) // 32 * 32
        ar.off += sz
        if ar.off > ar.limit:
            raise RuntimeError("SBUF arena overflow at %s" % name)


WBYTES = (2 * KC * DFF + FC * D) * 2


def ffn_walloc(sb):
    wg = sb.at("wg", [128, KC, DFF], BF16, SBASE)
    wu = sb.at("wu", [128, KC, DFF], BF16, SBASE + KC * DFF * 2)
    wd = sb.at("wd", [128, FC, D], BF16, SBASE + 2 * KC * DFF * 2)
    return wg, wu, wd


WCH = 4
FCH = [(0, 2), (2, 6), (6, 14), (14, 22)]


def wkey(tag, nm, f):
    for gi, (f0, f1) in enumerate(FCH):
        if f0 <= f < f1:
            return "%s%s%d" % (tag, nm, gi)
    raise ValueError


def load_ffn_weights(p, tag, W, wg_d, wu_d, wd_d, bg=False):
    wg, wu, wd = W
    wgv = wg_d.rearrange("(k p) f -> p k f", p=128)
    wuv = wu_d.rearrange("(k p) f -> p k f", p=128)
    wdv = wd_d.rearrange("(f p) d -> p f d", p=128)
    for gi, (f0, f1) in enumerate(FCH):
        c0, c1 = f0 * 128, f1 * 128
        p.op("pool", lambda e, c0=c0, c1=c1: e.dma_start(out=wg[:, :, c0:c1], in_=wgv[:, :, c0:c1]),
             writes=["%swg%d" % (tag, gi)], dma=True, semkey="%swg%d" % (tag, gi), bg=bg)
        p.op("pool", lambda e, c0=c0, c1=c1: e.dma_start(out=wu[:, :, c0:c1], in_=wuv[:, :, c0:c1]),
             writes=["%swu%d" % (tag, gi)], dma=True, semkey="%swu%d" % (tag, gi), bg=bg)
    for f in range(0, FC, 2):
        p.op("pool", lambda e, f=f: e.dma_start(out=wd[:, f:f + 2, :], in_=wdv[:, f:f + 2, :]), writes=[tag + "wd"],
             dma=True, semkey=tag + "wd", bg=bg)


def ffn_phase(p, sb, psb, tag, src, dst, W, gain_d, ones_d, ntiles):
    wg, wu, wd = W
    ar = Arena(sb, SBASE + WBYTES, SLIMIT)
    xt = [ar.alloc("x", [128, KC, TT], F32) for _ in range(2)]
    hT = ar.alloc("hT", [128, KC, TT], BF16)
    act = ar.alloc("act", [128, FC, TT], BF16)
    rstd = ar.alloc("rstd", [128, TT], F32)
    sg = [ar.alloc("sg", [128, TT], F32) for _ in range(2)]
    sq = [ar.alloc("sq", [128, TT], BF16) for _ in range(2)]
    ones_bf = ar.alloc("ones", [128, 128], BF16)
    gain = ar.alloc("gain", [128, KC], F32)

    K = lambda *a: (tag,) + a
    p.op("pool", lambda e: e.dma_start(out=ones_bf[:, :], in_=ones_d), writes=["consts"], dma=True,
         semkey=tag + "c")
    p.op("sp", lambda e: e.dma_start(out=gain[:, :], in_=gain_d), writes=["consts"], dma=True, semkey=tag + "c")
    xsrc = src.rearrange("(c p) n -> p c n", p=128)
    xdst = dst.rearrange("(c p) n -> p c n", p=128)

    def load_x(i):
        s = i % 2
        p.op("sp", lambda e: e.dma_start(out=xt[s][:, :, :], in_=xsrc[:, :, i * TT:(i + 1) * TT]),
             writes=[K("x", s)], dma=True, semkey=tag + "x%d" % s)

    def norm(i):
        rmsnorm_tile(p, xt[i % 2], K("x", i % 2), gain, hT, K("hT"), sq, [K("sq", 0), K("sq", 1)], ones_bf[:, :],
                     psb[7], ("ps", 7), rstd, K("rstd"), float(D), sq_eng="act")

    load_x(0)
    norm(0)
    for i in range(ntiles):
        s = i % 2
        if i + 1 < ntiles:
            load_x(i + 1)
        x = xt[s]
        for f in range(FC):
            gb, ub = psb[(f % 2) * 2], psb[(f % 2) * 2 + 1]
            gk, uk = ("ps", (f % 2) * 2), ("ps", (f % 2) * 2 + 1)
            for k in range(KC):
                mm(p, gb[:, :], [gk], wg[:, k, f * 128:(f + 1) * 128], hT[:, k, :], [K("hT"), wkey(tag, "wg", f)],
                   k == 0, k == KC - 1)
            for k in range(KC):
                mm(p, ub[:, :], [uk], wu[:, k, f * 128:(f + 1) * 128], hT[:, k, :], [K("hT"), wkey(tag, "wu", f)],
                   k == 0, k == KC - 1)
            sgt = sg[f % 2]
            p.op("act", lambda e, sgt=sgt, gb=gb: e.activation(sgt[:, :], gb[:, :], AF.Silu),
                 reads=[gk], writes=[K("sg", f % 2)])
            p.op("dve", lambda e, sgt=sgt, ub=ub, f=f: e.tensor_tensor(act[:, f, :], sgt[:, :], ub[:, :], ALU.mult),
                 reads=[K("sg", f % 2), uk], writes=[K("act", f)])
        if i + 1 < ntiles:
            norm(i + 1)
        for j in range(KC):
            yb = psb[4 + (j % 2)]
            yk = ("ps", 4 + (j % 2))
            for f in range(FC):
                mm(p, yb[:, :], [yk], wd[:, f, j * 128:(j + 1) * 128], act[:, f, :], [K("act", f), tag + "wd"],
                   f == 0, f == FC - 1)
            p.op("dve", lambda e, yb=yb, j=j, x=x: e.scalar_tensor_tensor(x[:, j, :], yb[:, :], 0.5, x[:, j, :],
                                                                          ALU.mult, ALU.add),
                 reads=[yk, K("x", s)], writes=[K("x", s)])
        p.op("act", lambda e, x=x, i=i: e.dma_start(out=xdst[:, :, i * TT:(i + 1) * TT], in_=x[:, :, :]),
             reads=[K("x", s)], writes=[K("xo", s)], dma=True, semkey=tag + "o%d" % s)


C_ONES, C_BLK64, C_NTRI8, C_NONES8, C_MSB, C_BTRI2, C_BLKU, C_ZERO, NCONST = 0, 128, 256, 384, 512, 640, 1152, 1280, 1408


def mix_phase(p, sb, psb, psb6, x1T, sbqT, sbkT, sbv_d, mixedT, w, ntiles, stop=99):
    tag = "m"
    K = lambda *a: (tag,) + a
    ar = Arena(sb, SBASE, SLIMIT)
    win = ar.alloc("win", [128, KC, DIN], BF16)
    wup = ar.alloc("wup", [32, 256], BF16)
    call = ar.alloc("call", [128, NCONST], BF16)
    cvec = ar.alloc("cvec", [128, 4], F32)
    gain = ar.alloc("gain", [128, KC], F32)
    xt = [ar.alloc("x", [128, KC, TT], F32) for _ in range(2)]
    hTs = [ar.alloc("hT", [128, KC, TT], BF16) for _ in range(2)]
    curh = {}
    sq = [ar.alloc("sq", [128, TT], BF16) for _ in range(2)]
    rstd = ar.alloc("rstd", [128, TT], F32)
    qg4 = ar.alloc("qg", [128, 2, 4, 128], F32)
    kg4 = ar.alloc("kg", [128, 2, 4, 128], F32)
    flat = lambda ap: ap.rearrange("p a b -> p (a b)")
    sgate = ar.alloc("sgate", [128, 4, TT], F32)
    lrT = ar.alloc("lrT", [32, TT], BF16)
    hsq = [ar.alloc("hsq", [128, TT], BF16) for _ in range(2)]
    hrstd = [ar.alloc("hrstd", [128, TT], F32) for _ in range(2)]
    sbqk = ar.alloc("sbqk", [128, 8, TT], BF16)
    ktok = ar.alloc("ktok", [128, 4, 256], F32)
    vtok = ar.alloc("vtok", [128, 4, 512], BF16)
    sbv = ar.alloc("sbv", [128, 4, 4, 128], BF16)
    esp = ar.alloc("esp", [128, 256], F32)
    sptok = ar.alloc("sptok", [128, 4, 256], BF16)
    qd = ar.alloc("qd", [128, 2, 4, 128], F32)
    kd = ar.alloc("kd", [128, 4, 128], F32)
    qdecP = [ar.alloc("qdecP", [128, 2, 4, 128], BF16) for _ in range(2)]
    kinv = ar.alloc("kinv", [128, 2, 4, 128], BF16)
    er = ar.alloc("er", [128, 256], F32)
    kendP = [[ar.alloc("kendP", [128, 4, 2, 2, 64], BF16) for _ in range(2)] for _ in range(2)]
    scm = [ar.alloc("scm", [128, 512], BF16) for _ in range(2)]
    S32 = ar.alloc("S32", [128, 2, 128], F32)
    Sbf = [ar.alloc("Sbf", [128, 2, 128], BF16) for _ in range(2)]
    o32_4 = ar.alloc("o32", [128, 4, 4, 128], F32)
    osq = [ar.alloc("osq", [128, TT], BF16) for _ in range(2)]
    orstd = ar.alloc("orstd", [128, TT], F32)
    omix = ar.alloc("omix", [128, 4, TT], BF16)

    p.op("pool", lambda e: e.dma_start(out=call[:, :], in_=w["c_all"]), writes=["mconsts"], dma=True, semkey="mc")
    p.op("sp", lambda e: e.dma_start(out=cvec[:, :], in_=w["c_vec"]), writes=["mconsts"], dma=True, semkey="mc")
    p.op("sp", lambda e: e.dma_start(out=gain[:, :], in_=w["mix_norm"]), writes=["mconsts"], dma=True, semkey="mc")
    p.op("pool", lambda e: e.dma_start(out=wup[0:16, :], in_=w["w_gk_up"]), writes=["wup"], dma=True, semkey="mc")
    p.op("pool", lambda e: e.dma_start(out=wup[16:17, :], in_=w["b_gk"]), writes=["wup"], dma=True, semkey="mc")
    winv = w["w_in"].rearrange("(k p) f -> p k f", p=128)
    for k in range(KC):
        p.op("pool", lambda e, k=k: e.dma_start(out=win[:, k, :], in_=winv[:, k, :]), writes=["win"], dma=True,
             semkey="mw")
    p.op("pool", lambda e: e.memset(lrT[:, :], 1.0), writes=[K("lrT")])
    for hh in range(2):
        p.op("pool", lambda e, hh=hh: e.memset(qdecP[hh][:, :, :, :], 0.0), writes=[K("qdec", 0), K("qdec", 1)])
        for half in range(2):
            p.op("pool", lambda e, hh=hh, half=half: e.memset(kendP[half][hh][:, :, :, :, :], 0.0),
                 writes=[K("kend", tb) for tb in range(4)])
    ones_bf = call[:, C_ONES:C_ONES + 128]
    blk64 = call[:, C_BLK64:C_BLK64 + 128]
    btri4 = call[:, C_BTRI2:C_BTRI2 + 512]
    btri = call[:, C_BTRI2:C_BTRI2 + 128]
    blku = call[:, C_BLKU:C_BLKU + 128]

    xsrc = x1T.rearrange("(c p) n -> p c n", p=128)
    sbq_v = sbqT.rearrange("(c p) n -> p c n", p=128)
    sbk_v = sbkT.rearrange("(c p) n -> p c n", p=128)
    sbv_v = sbv_d.rearrange("c p n f -> p c n f")
    mix_v = mixedT.rearrange("(c p) n -> p c n", p=128)

    def load_x(i):
        s = i % 2
        p.op("sp", lambda e: e.dma_start(out=xt[s][:, :, :], in_=xsrc[:, :, i * TT:(i + 1) * TT]),
             writes=[K("x", s)], dma=True, semkey="mx%d" % s)

    fmb = [0]

    def proj_fm(col, M):
        b = fmb[0] % 2
        fmb[0] += 1
        bank, bkey = psb[b], ("ps", b)
        for k in range(KC):
            mm(p, bank[0:M, :], [bkey], win[:, k, col:col + M], curh["hT"][:, k, :], [curh["hk"], "win"],
               k == 0, k == KC - 1)
        return bank, bkey

    tmb = [0]

    def proj_tm(tb, col, N):
        b = 3 + tmb[0] % 2
        tmb[0] += 1
        bank, bkey = psb[b], ("ps", b)
        for k in range(KC):
            mm(p, bank[:, 0:N], [bkey], curh["hT"][:, k, tb * 128:(tb + 1) * 128], win[:, k, col:col + N],
               [curh["hk"], "win"], k == 0, k == KC - 1)
        return bank, bkey

    def norm(i):
        rmsnorm_tile(p, xt[i % 2], K("x", i % 2), gain, hTs[i % 2], K("hT", i % 2), sq, [K("sq", 0), K("sq", 1)],
                     ones_bf, psb[2], ("ps", 2), rstd, K("rstd"), float(D), ckey="mconsts", sq_eng="act")

    load_x(0)
    norm(0)
    nsc = 0
    chunk_ctr = 0
    for i in range(ntiles):
        s = i % 2
        cols = slice(i * TT, (i + 1) * TT)
        if i + 1 < ntiles:
            load_x(i + 1)
        curh["hT"], curh["hk"] = hTs[i % 2], K("hT", i % 2)
        bank, bkey = proj_fm(1536, 16)
        p.op("act", lambda e, bank=bank: e.copy(lrT[0:16, :], bank[0:16, :]), reads=[bkey], writes=[K("lrT")])
        for pr in range(2):
            bank, bkey = proj_fm(pr * 128, 128)
            p.op("act", lambda e, bank=bank, pr=pr: e.copy(flat(qg4[:, pr, :, :]), bank[:, :]), reads=[bkey], writes=[K("qg")])
            bank, bkey = proj_fm(256 + pr * 128, 128)
            p.op("dve", lambda e, bank=bank, pr=pr: e.tensor_copy(flat(kg4[:, pr, :, :]), bank[:, :]), reads=[bkey],
                 writes=[K("kg")])
        if stop <= 1:
            continue
        for tb in range(4):
            bank, bkey = proj_tm(tb, 256, 512)
            p.op("dve", lambda e, bank=bank, tb=tb: e.tensor_copy(ktok[:, tb, :], bank[:, 0:256]), reads=[bkey],
                 writes=[K("ktok", tb)])
            p.op("dve", lambda e, bank=bank, tb=tb: e.tensor_copy(vtok[:, tb, 0:256], bank[:, 256:512]),
                 reads=[bkey], writes=[K("vtok", tb)])
            bank, bkey = proj_tm(tb, 768, 256)
            p.op("act", lambda e, bank=bank, tb=tb: e.copy(vtok[:, tb, 256:512], bank[:, 0:256]),
                 reads=[bkey], writes=[K("vtok", tb)])
            bank, bkey = proj_tm(tb, 2576, 512)
            p.op("act", lambda e, bank=bank, tb=tb: e.copy(sbv[:, :, tb, :],
                                                           bank[:, :].rearrange("p (c f) -> p c f", c=4)),
                 reads=[bkey], writes=[K("sbv")])
        p.op("sp", lambda e, i=i: e.dma_start(out=sbv_v[:, :, i * 4:(i + 1) * 4, :], in_=sbv[:, :, :, :]),
             reads=[K("sbv")], writes=[K("sbv_o")], dma=True, semkey="mosbv")
        if i + 1 < ntiles:
            norm(i + 1)
        if stop <= 2:
            continue
        for tb in range(4):
            la = psb[5]
            mm(p, la[:, 0:256], [("ps", 5)], lrT[0:17, tb * 128:(tb + 1) * 128], wup[0:17, :],
               [K("lrT"), "wup"], True, True)
            p.op("act", lambda e, la=la: e.activation(esp[:, :], la[:, 0:256], AF.Exp, scale=-1.0),
                 reads=[("ps", 5)], writes=[K("esp")])
            p.op("act", lambda e, tb=tb: e.activation(sptok[:, tb, :], esp[:, :], AF.Ln, bias=1.0),
                 reads=[K("esp")], writes=[K("sptok", tb)])
            mm(p, la[:, 256:512], [("ps", 5)], blku, sptok[:, tb, :], [K("sptok", tb), "mconsts"], True, True)
            p.op("act", lambda e, la=la: e.activation(er[:, :], la[:, 256:512], AF.Exp, scale=-1.0 / 16),
                 reads=[("ps", 5)], writes=[K("er")])
            for half in range(2):
                trow = slice(half * 64, (half + 1) * 64)
                for hh in range(2):
                    p.op("dve", lambda e, tb=tb, half=half, hh=hh, trow=trow: e.tensor_tensor(
                        kendP[half][hh][trow, tb, :, hh, :],
                        ktok[trow, tb, :].rearrange("p (a b c) -> p a b c", a=2, b=2)[:, :, hh, :],
                        er[trow, :].rearrange("p (a b c) -> p a b c", a=2, b=2)[:, :, hh, :], ALU.mult),
                        reads=[K("ktok", tb), K("er")], writes=[K("kend", tb)])
        for pr in range(2):
            for tb in range(4):
                mm(p, psb6[:, tb, :], [("ps", 6)], sptok[:, tb, pr * 128:(pr + 1) * 128], btri,
                   [K("sptok", tb), "mconsts"], True, True)
            p.op("act", lambda e, pr=pr: e.activation(qd[:, pr, :, :], psb6[:, :, :], AF.Exp, scale=-1.0 / 16),
                 reads=[("ps", 6)], writes=[K("qd", pr)])
            p.op("act", lambda e: e.activation(kd[:, :, :], psb6[:, :, :], AF.Exp, scale=1.0 / 16),
                 reads=[("ps", 6)], writes=[K("kd")])
            for hh in range(2):
                rows = slice(hh * 64, (hh + 1) * 64)
                p.op("dve", lambda e, pr=pr, hh=hh, rows=rows: e.scalar_tensor_tensor(
                    qdecP[hh][rows, pr, :, :], qg4[rows, pr, :, :], 0.125, qd[rows, pr, :, :], ALU.mult, ALU.mult),
                    reads=[K("qg"), K("qd", pr)], writes=[K("qdec", pr)])
            p.op("dve", lambda e, pr=pr: e.tensor_tensor(kinv[:, pr, :, :], kg4[:, pr, :, :], kd[:, :, :], ALU.mult),
                 reads=[K("kg"), K("kd")], writes=[K("kinv", pr)])
        if stop <= 3:
            continue
        for h in range(4):
            bank, bkey = proj_fm(1024 + h * 128, 128)
            p.op("act", lambda e, bank=bank, h=h: e.activation(sgate[:, h, :], bank[:, :], AF.Silu), reads=[bkey],
                 writes=[K("sgate", h)])
        if stop <= 4:
            continue
        if i % (SEQ // TT) == 0:
            p.op("pool", lambda e: e.memset(S32[:, :, :], 0.0), writes=[K("S32")])
            p.op("pool", lambda e, cc=chunk_ctr: e.memset(Sbf[cc % 2][:, :, :], 0.0), writes=[K("Sbf", chunk_ctr % 2)])
        for tb in range(4):
            scb = scm[nsc % 2]
            sck = K("scm", nsc % 2)
            nsc += 1
            for pr in range(2):
                for hh in range(2):
                    h = 2 * pr + hh
                    mm(p, psb[7][:, h * 128:(h + 1) * 128], [("ps", 7)], kinv[:, pr, tb, :],
                       qdecP[hh][:, pr, tb, :], [K("kinv", pr), K("qdec", pr)], True, True)
            p.op("dve", lambda e, scb=scb: e.tensor_tensor(scb[:, :], psb[7][:, :], btri4, ALU.mult),
                 reads=[("ps", 7), "mconsts"], writes=[sck])
            for h in range(4):
                mm(p, psb6[:, h, :], [("ps", 6)], vtok[:, tb, h * 128:(h + 1) * 128],
                   scb[:, h * 128:(h + 1) * 128], [K("vtok", tb), sck], h == 0, False)
            for half in range(2):
                for pr in range(2):
                    reg = (half * 2 + pr) * 128
                    for hh in range(2):
                        h = 2 * pr + hh
                        mm(p, psb[5][:, reg:reg + 128], [("ps", 5)],
                           kendP[half][hh][:, tb, pr, :, :].rearrange("p a b -> p (a b)"),
                           vtok[:, tb, h * 128:(h + 1) * 128], [K("kend", tb), K("vtok", tb)], hh == 0, hh == 1)
            for half in range(2):
                n = chunk_ctr + tb * 2 + half
                cur, nxt = n % 2, (n + 1) % 2
                for pr in range(2):
                    for hh in range(2):
                        h = 2 * pr + hh
                        mm(p, psb6[:, h, half * 64:(half + 1) * 64], [("ps", 6)], Sbf[cur][:, pr, :],
                           qdecP[hh][:, pr, tb, half * 64:(half + 1) * 64], [K("Sbf", cur), K("qdec", pr)],
                           False, half == 1)
                for pr in range(2):
                    reg = (half * 2 + pr) * 128
                    p.op("dve", lambda e, pr=pr, tb=tb, half=half, reg=reg:
                         e.scalar_tensor_tensor(S32[:, pr, :], S32[:, pr, :],
                                                qd[:, pr, tb, half * 64 + 63:half * 64 + 64],
                                                psb[5][:, reg:reg + 128], ALU.mult, ALU.add),
                         reads=[K("S32"), K("qd", pr), ("ps", 5)], writes=[K("S32")])
                p.op("act", lambda e, nxt=nxt: e.copy(Sbf[nxt][:, :, :], S32[:, :, :]),
                     reads=[K("S32")], writes=[K("Sbf", nxt)])
            p.op("act", lambda e, tb=tb: e.copy(o32_4[:, :, tb, :], psb6[:, :, :]), reads=[("ps", 6)],
                 writes=[K("o32")])
        chunk_ctr += 8
        if stop <= 5:
            continue
        for h in range(4):
            oq = osq[h % 2]
            p.op("act", lambda e, oq=oq, h=h: e.activation(oq[:, :], flat(o32_4[:, h, :, :]), AF.Square),
                 reads=[K("o32")], writes=[K("osq", h % 2)])
            mm(p, psb[2][:, :], [("ps", 2)], ones_bf, oq[:, :], [K("osq", h % 2), "mconsts"], True, True)
            p.op("act", lambda e: e.activation(orstd[:, :], psb[2][:, :], AF.Ln, bias=EPS, scale=1.0 / 128),
                 reads=[("ps", 2)], writes=[K("orstd")])
            p.op("act", lambda e: e.activation(orstd[:, :], orstd[:, :], AF.Exp, scale=-0.5),
                 reads=[K("orstd")], writes=[K("orstd")])
            p.op("dve", lambda e, h=h: e.scalar_tensor_tensor(flat(o32_4[:, h, :, :]), flat(o32_4[:, h, :, :]), cvec[:, 2:3],
                                                               orstd[:, :], ALU.mult, ALU.mult),
                 reads=[K("o32"), K("orstd"), "mconsts"], writes=[K("o32")])
            p.op("dve", lambda e, h=h: e.tensor_tensor(omix[:, h, :], flat(o32_4[:, h, :, :]), sgate[:, h, :], ALU.mult),
                 reads=[K("o32"), K("sgate", h)], writes=[K("omix")])
        p.op("sp", lambda e, cols=cols: e.dma_start(out=mix_v[:, 0:4, cols], in_=omix[:, :, :]),
             reads=[K("omix")], writes=[K("omix_o")], dma=True, semkey="moomix")
        if stop <= 6:
            continue
        for idx in range(8):
            col = (1552 if idx < 4 else 2064) + (idx % 4) * 128
            bank, bkey = proj_fm(col, 128)
            hq, hr = hsq[idx % 2], hrstd[idx % 2]
            p.op("act", lambda e, hq=hq, bank=bank: e.activation(hq[:, :], bank[:, :], AF.Square), reads=[bkey],
                 writes=[K("hsq", idx % 2)])
            mm(p, psb[2][:, :], [("ps", 2)], blk64, hq[:, :], [K("hsq", idx % 2), "mconsts"], True, True)
            p.op("act", lambda e, hr=hr: e.activation(hr[:, :], psb[2][:, :], AF.Ln, bias=EPS, scale=1.0 / 64),
                 reads=[("ps", 2)], writes=[K("hrstd", idx % 2)])
            p.op("act", lambda e, hr=hr: e.activation(hr[:, :], hr[:, :], AF.Exp, scale=-0.5),
                 reads=[K("hrstd", idx % 2)], writes=[K("hrstd", idx % 2)])
            gcol = 0 if idx < 4 else 1
            p.op("dve", lambda e, idx=idx, bank=bank, hr=hr, gcol=gcol:
                 e.scalar_tensor_tensor(sbqk[:, idx, :], bank[:, :], cvec[:, gcol:gcol + 1], hr[:, :],
                                        ALU.mult, ALU.mult),
                 reads=[bkey, K("hrstd", idx % 2), "mconsts"], writes=[K("sbqk")])
        p.op("sp", lambda e, cols=cols: e.dma_start(out=sbq_v[:, :, cols], in_=sbqk[:, 0:4, :]),
             reads=[K("sbqk")], writes=[K("sbqk_o")], dma=True, semkey="moqk")
        p.op("sp", lambda e, cols=cols: e.dma_start(out=sbk_v[:, :, cols], in_=sbqk[:, 4:8, :]),
             reads=[K("sbqk")], writes=[K("sbqk_o")], dma=True, semkey="moqk")


def sb_phase(p, sb, psb, sbqT, sbkT, sbv_d, mixedT, w, nseq, nsblk=8, npairs=4, after_consts=None):
    tag = "s"
    K = lambda *a: (tag,) + a
    ar = Arena(sb, SBASE + WBYTES, SLIMIT)
    call = ar.alloc("call", [128, NCONST], BF16)
    qT = [ar.alloc("qT", [128, SEQ], BF16) for _ in range(2)]
    kT = [ar.alloc("kT", [128, SEQ], BF16) for _ in range(2)]
    vv1 = ar.alloc("vv", [128, SEQ // 128, 128], BF16)
    vv = [vv1, vv1]
    ets = [[ar.alloc("et", [128, TT], F32) for _ in range(2)] for _ in range(2)]
    St = [[ar.alloc("St", [128, TT], BF16) for _ in range(2)] for _ in range(3)]
    Ssum = [[[ar.alloc("Ssum", [128, TT], BF16) for _ in range(2)] for _ in range(2)] for _ in range(2)]
    wt = [[ar.alloc("wt", [128, TT], BF16) for _ in range(2)] for _ in range(3)]
    ob = [ar.alloc("ob", [128, TT], BF16) for _ in range(2)]
    p.op("pool", lambda e: e.dma_start(out=call[:, :], in_=w["c_all"]), writes=["sconsts"], dma=True, semkey="sc")
    if after_consts is not None:
        after_consts()
    ntri8 = call[:, C_NTRI8:C_NTRI8 + 128]
    nones8 = call[:, C_NONES8:C_NONES8 + 128]
    msb = call[:, C_MSB:C_MSB + 128]
    zero = call[:, C_ZERO:C_ZERO + 128]
    obanks = [psb[6][:, :, :].rearrange("p a b -> p (a b)"), psb[7][:, :]]
    okeys = [("ps", 6), ("ps", 7)]
    tasks = []
    it = 0
    nsb = 0
    for b in range(nseq):
        for c in range(npairs):
            s = it % 2
            it += 1
            for i in range(nsblk):
                nb = 4 * i + 4
                for t in range(nb):
                    tasks.append(dict(b=b, c=c, s=s, i=i, t=t, kb=4 * i + 3 - t, nb=nb, nsb=nsb,
                                      load=(i == 0 and t == 0)))
                nsb += 1
    G = len(tasks)

    def load_vv(b, c):
        p.op("sp", lambda e: e.dma_start(out=vv1[:, :, :], in_=sbv_d[c, :, b * 32:(b + 1) * 32, :]),
             writes=[K("vv")], dma=True, semkey="sv")

    def bank(g, hh):
        k = (g % 3) * 2 + hh
        return psb[k], ("ps", k)

    def stage1(g):
        T = tasks[g]
        b, c, s, i, t, kb = T["b"], T["c"], T["s"], T["i"], T["t"], T["kb"]
        par = T["nsb"] % 2
        if T["load"]:
            tcols = slice(b * SEQ, (b + 1) * SEQ)
            p.op("sp", lambda e: e.dma_start(out=qT[s][:, :], in_=sbqT[c * 128:(c + 1) * 128, tcols]),
                 writes=[K("qT", s)], dma=True, semkey="sq%d" % s)
            p.op("sp", lambda e: e.dma_start(out=kT[s][:, :], in_=sbkT[c * 128:(c + 1) * 128, tcols]),
                 writes=[K("kT", s)], dma=True, semkey="sq%d" % s)
            if g == 0:
                load_vv(b, c)
        if t == 0:
            mm(p, obanks[par], [okeys[par]], zero, qT[s][:, 0:512], ["sconsts", K("qT", s)], True, False)
            for hh in range(2):
                for j in range(2):
                    p.op("dve", lambda e, hh=hh, j=j: e.memset(Ssum[par][hh][j][:, :], 0.0),
                         writes=[K("Ssum", par, hh, j)])
        r = kb - 4 * i
        c0 = 128 * max(r, 0)
        T["c0"], T["diag"] = c0, r >= 0
        q0, q1 = i * 512 + c0, (i + 1) * 512
        for hh in range(2):
            rows = slice(hh * 64, (hh + 1) * 64)
            A, Ak = bank(g, hh)
            mm(p, A[:, c0:512], [Ak], kT[s][rows, kb * 128:(kb + 1) * 128], qT[s][rows, q0:q1],
               [K("kT", s), K("qT", s)], True, False)
        for hh in range(2):
            A, Ak = bank(g, hh)
            et = ets[g % 2][hh]
            p.op("act", lambda e, et=et, A=A: e.activation(et[:, c0:512], A[:, c0:512], AF.Exp, scale=0.125),
                 reads=[Ak], writes=[K("et", g % 2, hh)])

    def stage1b(g):
        T = tasks[g]
        t, c0, par = T["t"], T["c0"], T["nsb"] % 2
        for hh in range(2):
            et = ets[g % 2][hh]
            S_t, Sk = St[g % 3][hh], K("St", g % 3, hh)
            p.op("act", lambda e, et=et, S_t=S_t: e.activation(S_t[:, c0:512], et[:, c0:512], AF.Ln, bias=1.0),
                 reads=[K("et", g % 2, hh)], writes=[Sk])
            if T["diag"]:
                p.op("dve", lambda e, S_t=S_t: e.tensor_tensor(S_t[:, c0:c0 + 128], S_t[:, c0:c0 + 128], msb,
                                                               ALU.mult),
                     reads=[Sk, "sconsts"], writes=[Sk])
            if t + 1 < T["nb"]:
                src, dst = Ssum[par][hh][t % 2], Ssum[par][hh][(t + 1) % 2]
                p.op("dve", lambda e, src=src, dst=dst, S_t=S_t: e.tensor_tensor(dst[:, c0:512], src[:, c0:512],
                                                                                 S_t[:, c0:512], ALU.add),
                     reads=[Sk, K("Ssum", par, hh, t % 2)], writes=[K("Ssum", par, hh, (t + 1) % 2)])

    def stage2(g):
        T = tasks[g]
        t, c0, par = T["t"], T["c0"], T["nsb"] % 2
        for hh in range(2):
            A, Ak = bank(g, hh)
            S_t, Sk = St[g % 3][hh], K("St", g % 3, hh)
            mm(p, A[:, c0:512], [Ak], ntri8, S_t[:, c0:512], [Sk, "sconsts"], False, t == 0)
            if t > 0:
                mm(p, A[:, c0:512], [Ak], nones8, Ssum[par][hh][t % 2][:, c0:512],
                   [K("Ssum", par, hh, t % 2), "sconsts"], False, True)
        for hh in range(2):
            A, Ak = bank(g, hh)
            w_t, wk = wt[g % 3][hh], K("wt", g % 3, hh)
            p.op("act", lambda e, w_t=w_t, A=A: e.activation(w_t[:, c0:512], A[:, c0:512], AF.Exp, scale=0.125),
                 reads=[Ak], writes=[wk])
            if T["diag"]:
                p.op("dve", lambda e, w_t=w_t: e.tensor_tensor(w_t[:, c0:c0 + 128], w_t[:, c0:c0 + 128], msb,
                                                               ALU.mult),
                     reads=[wk, "sconsts"], writes=[wk])

    def stage3(g):
        T = tasks[g]
        b, c, s, i, t, kb, c0 = T["b"], T["c"], T["s"], T["i"], T["t"], T["kb"], T["c0"]
        par = T["nsb"] % 2
        obank, okey = obanks[par], okeys[par]
        for hh in range(2):
            w_t, wk = wt[g % 3][hh], K("wt", g % 3, hh)
            lhsT = vv[s][:, kb, hh * 64:(hh + 1) * 64]
            out = obank[hh * 64:(hh + 1) * 64, c0:512]
            rhs = w_t[:, c0:512]
            p.op("pe", lambda e, out=out, lhsT=lhsT, rhs=rhs, hh=hh: e.matmul(
                out, lhsT, rhs, start=False, stop=(kb == 0), skip_group_check=True, tile_position=(0, hh * 64)),
                reads=[K("vv"), wk], writes=[okey])
        if kb == 0:
            o_s, osk = ob[par], K("ob", par)
            p.op("dve", lambda e: e.tensor_copy(o_s[:, :], obank), reads=[okey], writes=[osk])
            tok0 = b * SEQ + i * 512
            p.op("sp", lambda e: e.dma_start(out=mixedT[512 + c * 128:512 + (c + 1) * 128, tok0:tok0 + 512],
                                             in_=o_s[:, :]),
                 reads=[osk], writes=[osk + ("o",)], dma=True, semkey="so%d" % par)

    for step in range(G + 2):
        if step < G:
            stage1(step)
        if 0 <= step - 1 < G:
            stage2(step - 1)
        if step < G:
            stage1b(step)
        if 0 <= step - 2 < G:
            stage3(step - 2)
            g3 = step - 2
            if g3 + 1 < G and tasks[g3 + 1]["load"]:
                load_vv(tasks[g3 + 1]["b"], tasks[g3 + 1]["c"])


def wout_phase(p, sb, psb, x1T, mixedT, w, ntiles):
    tag = "o"
    K = lambda *a: (tag,) + a
    ar = Arena(sb, SBASE + WBYTES, SLIMIT)
    wout = ar.alloc("wout", [128, KC, D], BF16)
    xt = [ar.alloc("x", [128, KC, TT], F32) for _ in range(2)]
    mt = [ar.alloc("mt", [128, KC, TT], BF16) for _ in range(2)]
    wv = w["w_out"].rearrange("(k p) f -> p k f", p=128)
    for k in range(0, KC, 2):
        p.op("pool", lambda e, k=k: e.dma_start(out=wout[:, k:k + 2, :], in_=wv[:, k:k + 2, :]), writes=["wout"],
             dma=True, semkey="ow")
    xv = x1T.rearrange("(c p) n -> p c n", p=128)
    mv = mixedT.rearrange("(c p) n -> p c n", p=128)

    def load(i):
        s = i % 2
        cols = slice(i * TT, (i + 1) * TT)
        p.op("sp", lambda e: e.dma_start(out=xt[s][:, :, :], in_=xv[:, :, cols]), writes=[K("x", s)], dma=True,
             semkey="ox%d" % s)
        p.op("sp", lambda e: e.dma_start(out=mt[s][:, :, :], in_=mv[:, :, cols]), writes=[K("m", s)], dma=True,
             semkey="ox%d" % s)

    load(0)
    for i in range(ntiles):
        s = i % 2
        if i + 1 < ntiles:
            load(i + 1)
        for j in range(KC):
            b = j % 4
            for c in range(KC):
                mm(p, psb[b][:, :], [("ps", b)], wout[:, c, j * 128:(j + 1) * 128], mt[s][:, c, :],
                   [K("m", s), "wout"], c == 0, c == KC - 1)
            p.op("dve", lambda e, b=b, j=j, s=s: e.tensor_tensor(xt[s][:, j, :], xt[s][:, j, :], psb[b][:, :], ALU.add),
                 reads=[("ps", b), K("x", s)], writes=[K("x", s)])
        p.op("act", lambda e, s=s, i=i: e.dma_start(out=xv[:, :, i * TT:(i + 1) * TT], in_=xt[s][:, :, :]),
             reads=[K("x", s)], writes=[K("xo", s)], dma=True, semkey="oo%d" % s)


def build(cfg):
    from contextlib import ExitStack
    nc = bass.Bass("TRN2", target_bir_lowering=False)
    dt = lambda name, shape, dtype=F32, kind="ExternalInput": nc.dram_tensor(name, list(shape), dtype, kind=kind).ap()
    xT = dt("xT", [D, NTOK])
    yT = dt("yT", [D, NTOK], kind="ExternalOutput")
    w = {}
    for nm, shp in (("ffn1_w_gate", [D, DFF]), ("ffn1_w_up", [D, DFF]), ("ffn1_w_down", [DFF, D]),
                    ("ffn2_w_gate", [D, DFF]), ("ffn2_w_up", [D, DFF]), ("ffn2_w_down", [DFF, D]),
                    ("w_in", [D, DIN]), ("w_out", [D, D]), ("w_gk_up", [16, 256]), ("b_gk", [1, 256]),
                    ("ffn1_norm", [128, KC]), ("mix_norm", [128, KC]), ("ffn2_norm", [128, KC]),
                    ("c_vec", [128, 4]), ("c_ones", [128, 128]), ("c_all", [128, NCONST])):
        w[nm] = dt(nm, shp)
    dbg = cfg.get("debug", False)
    skind = "ExternalOutput" if dbg else "Internal"
    x1T = dt("x1T", [D, NTOK], F32, kind=skind)
    sbqT = dt("sbqT", [512, NTOK], BF16, kind=skind)
    sbkT = dt("sbkT", [512, NTOK], BF16, kind=skind)
    sbv_d = dt("sbv", [4, 128, NTOK // 128, 128], BF16, kind=skind)
    mixedT = dt("mixedT", [D, NTOK], BF16, kind=skind)
    sb = SB(nc)
    psb = [nc.alloc_psum_tensor("psb%d" % i, [128, 512], F32) for i in range(6)]
    psb6 = nc.alloc_psum_tensor("psb6", [128, 4, 128], F32)
    psb7 = nc.alloc_psum_tensor("psb7", [128, 512], F32)
    p = Prog(nc)
    ntiles = cfg.get("ntiles", NTOK // TT)
    phases = cfg.get("phases", "1234")
    W = ffn_walloc(sb)

    pb = psb + [psb6, psb7]
    if "1" in phases:
        load_ffn_weights(p, "f1", W, w["ffn1_w_gate"], w["ffn1_w_up"], w["ffn1_w_down"])
        ffn_phase(p, sb, pb, "f1", xT, x1T if len(phases) > 1 else yT, W, w["ffn1_norm"], w["c_ones"], ntiles)
        p.barrier()
    if "2" in phases:
        mix_phase(p, sb, pb, psb6, x1T, sbqT, sbkT, sbv_d, mixedT, w, ntiles, stop=cfg.get('mixstop', 99))
        p.barrier()
    if "3" in phases:
        sb_phase(p, sb, pb, sbqT, sbkT, sbv_d, mixedT, w, cfg.get("nseq", 2), cfg.get("nsblk", 8),
                 cfg.get("npairs", 4),
                 after_consts=lambda: load_ffn_weights(p, "f2", W, w["ffn2_w_gate"], w["ffn2_w_up"],
                                                       w["ffn2_w_down"], bg=True))
        p.barrier()
    if "4" in phases:
        wout_phase(p, sb, pb, x1T, mixedT, w, ntiles)
        p.barrier()
        ffn_phase(p, sb, pb, "f2", x1T, yT, W, w["ffn2_norm"], w["c_ones"], ntiles)
    with ExitStack() as stack:
        p.emit(stack)
    return nc


def host_consts():
    idx = np.arange(128)
    j, t = idx[:, None], idx[None, :]
    same = (j // 64) == (t // 64)
    call = np.zeros((128, NCONST), np.float32)
    call[:, C_ONES:C_ONES + 128] = 1.0
    call[:, C_BLK64:C_BLK64 + 128] = same
    call[:, C_NTRI8:C_NTRI8 + 128] = np.where(j >= t, -8.0, 0.0)
    call[:, C_NONES8:C_NONES8 + 128] = -8.0
    call[:, C_MSB:C_MSB + 128] = (j < t)
    bt = (same & (j <= t)).astype(np.float32)
    for rep in range(4):
        call[:, C_BTRI2 + rep * 128:C_BTRI2 + (rep + 1) * 128] = bt
    call[:, C_BLKU:C_BLKU + 128] = (same & (j > t))
    return {"c_ones": np.ones((128, 128), np.float32), "c_all": call}


def kernel(**inputs):
    cfg = inputs.pop("_cfg", {})
    x = np.asarray(inputs["x"], dtype=np.float32)
    B, T, Dm = x.shape
    xs = x.reshape(NCORES, NTOK, Dm)
    consts = host_consts()
    shared = {}
    for k, v in inputs.items():
        if k == "x":
            continue
        a = np.ascontiguousarray(np.asarray(v, dtype=np.float32))
        a = a.reshape(a.shape[1:])
        if k == "b_gk":
            a = a.reshape(1, 256)
        if k in ("ffn1_norm", "mix_norm", "ffn2_norm"):
            a = np.ascontiguousarray(a.reshape(KC, 128).T)
        shared[k] = a
    cvec = np.zeros((128, 4), np.float32)
    cvec[:, 0] = np.tile(shared.pop("sb_q_norm").reshape(64), 2)
    cvec[:, 1] = np.tile(shared.pop("sb_k_norm").reshape(64), 2)
    cvec[:, 2] = shared.pop("gla_out_norm").reshape(128)
    shared["c_vec"] = cvec
    shared.update(consts)
    in_maps = []
    for c in range(NCORES):
        m = dict(shared)
        m["xT"] = np.ascontiguousarray(xs[c].T)
        in_maps.append(m)
    nc = build(cfg)
    if cfg.get("trace"):
        res = run_bass_kernel_spmd(nc, in_maps, core_ids=list(range(NCORES)), trace=True)
        print("EXEC_TIME_NS", res.exec_time_ns)
    else:
        res = run_bass_kernel_spmd(nc, in_maps, core_ids=list(range(NCORES)))
    if cfg.get("raw"):
        return res
    out = np.empty((NCORES, NTOK, Dm), np.float32)
    for c in range(NCORES):
        out[c] = res.results[c]["yT"].T
    return out.reshape(B, T, Dm)
```

```python
import numpy as np
import concourse.bass as bass
import concourse.mybir as mybir
from concourse.bass_utils import run_bass_kernel_spmd

F32 = mybir.dt.float32
BF16 = mybir.dt.bfloat16
AF = mybir.ActivationFunctionType
ALU = mybir.AluOpType

NCORES = 8
D = 1024
DFF = 2816
NTOK = 8192
SEQ = 4096
TT = 512
KC = D // 128
FC = DFF // 128
DIN = 3088
EPS = 1e-6

SEM_CAP = 30000
import os
SBV_MODE = int(os.environ.get('SBV_MODE', '2'))
SBASE = 16512
SLIMIT = 229376


class Op:
    __slots__ = ("eng", "fn", "deps", "dma", "semkey", "need_inc", "inc", "waits", "idx", "bg")


class Prog:
    def __init__(self, nc):
        self.nc = nc
        self.ops = []
        self.last_w = {}
        self.readers = {}
        self.last_on_eng = {}
        self.last_dma_key = {}
        self.pending_bar = {}

    def op(self, eng, fn, reads=(), writes=(), dma=False, semkey=None, bg=False):
        o = Op()
        o.eng, o.fn, o.dma, o.semkey, o.bg = eng, fn, dma, semkey, bg
        o.idx = len(self.ops)
        o.need_inc = False
        o.inc = None
        o.waits = None
        deps = {}
        for k in reads:
            w = self.last_w.get(k)
            if w is not None:
                deps[w] = True
            if isinstance(k, tuple) and k[0] == "ps":
                for r in self.readers.get(k, ()):
                    if self.ops[r].eng != eng and r not in deps:
                        deps[r] = False
        for k in writes:
            w = self.last_w.get(k)
            if w is not None and w not in deps:
                deps[w] = False
            for r in self.readers.get(k, ()):
                if r not in deps:
                    deps[r] = False
        bar = self.pending_bar.pop(eng, None)
        if bar:
            for b in bar:
                deps[b] = True
        for k in reads:
            self.readers.setdefault(k, []).append(o.idx)
        for k in writes:
            self.last_w[k] = o.idx
            self.readers[k] = []
        o.deps = deps
        self.ops.append(o)
        self.last_on_eng[eng if not dma else ("dmaq", eng)] = o.idx
        if dma:
            if semkey is None:
                raise ValueError("dma needs semkey")
            o.semkey = semkey = "%s_%s" % (semkey, eng)
            if not bg:
                self.last_dma_key[semkey] = o.idx
        return o

    def barrier(self):
        lasts = [v for k, v in self.last_on_eng.items() if not isinstance(k, tuple)]
        lasts += list(self.last_dma_key.values())
        for e in ("pe", "act", "dve", "pool", "sp"):
            self.pending_bar[e] = list(lasts)

    def emit(self, stack):
        nc = self.nc
        ops = self.ops
        for o in ops:
            for d, raw in o.deps.items():
                od = ops[d]
                if od.dma:
                    continue
                if od.eng == o.eng and not o.dma:
                    if o.eng == "pe":
                        continue
                od.need_inc = True
        sems = {}

        def get_sem(name):
            if name not in sems:
                sems[name] = stack.enter_context(nc.semaphore(name))
            return sems[name]

        eng_cnt = {}
        dma_cnt = {}
        waited = {}
        boundary = {}
        dma_sem_eng = {}
        for o in ops:
            waits = []
            if o.dma:
                bv = boundary.get("d_%s" % (o.semkey,), 0)
                if bv > 0:
                    waits.append(("d_%s" % (o.semkey,), bv))
            for d, raw in o.deps.items():
                od = ops[d]
                if od.dma:
                    waits.append((od.inc[0], dma_cnt[od.semkey][1]))
                    continue
                if od.eng == o.eng and not o.dma:
                    if o.eng == "pe":
                        continue
                waits.append(od.inc)
            best = {}
            for s, v in waits:
                if v > best.get(s, 0):
                    best[s] = v
                if s.startswith("d_") and v > boundary.get(s, 0):
                    boundary[s] = v
            wl = []
            for s, v in best.items():
                key = (o.eng, s)
                if waited.get(key, 0) >= v:
                    continue
                waited[key] = v
                wl.append((s, v))
            o.waits = wl
            if o.dma:
                ep, cnt = dma_cnt.get(o.semkey, (None, 0))
                if ep is None:
                    ep = "d_%s" % (o.semkey,)
                    get_sem(ep)
                    cnt = 0
                assert cnt + 16 <= 60000
                cnt += 16
                dma_cnt[o.semkey] = (ep, cnt)
                dma_sem_eng[ep] = o.eng
                o.inc = (ep, cnt)
            elif o.need_inc:
                ep, cnt = eng_cnt.get(o.eng, (None, 0))
                if ep is None or cnt + 1 > SEM_CAP:
                    n = sum(1 for k in sems if k.startswith("e_%s_" % o.eng))
                    ep = "e_%s_%d" % (o.eng, n)
                    get_sem(ep)
                    cnt = 0
                cnt += 1
                eng_cnt[o.eng] = (ep, cnt)
                o.inc = (ep, cnt)
        final_waits = {}
        for (ep, cnt) in dma_cnt.values():
            final_waits.setdefault(dma_sem_eng[ep], []).append((ep, cnt))
        by_eng = {e: [] for e in ("pe", "act", "dve", "pool", "sp")}
        for o in ops:
            by_eng[o.eng].append(o)

        def run(engname, eng):
            for o in by_eng[engname]:
                for s, v in o.waits:
                    eng.wait_ge(sems[s], v)
                ins = o.fn(eng)
                if o.inc is not None:
                    ins.then_inc(sems[o.inc[0]], 16 if o.dma else 1)
            for s, v in final_waits.get(engname, ()):
                eng.wait_ge(sems[s], v)

        with nc.Block() as block:
            @block.tensor
            def _(e):
                run("pe", e)

            @block.scalar
            def _(e):
                run("act", e)

            @block.vector
            def _(e):
                run("dve", e)

            @block.gpsimd
            def _(e):
                run("pool", e)

            @block.sync
            def _(e):
                run("sp", e)


class SB:
    def __init__(self, nc):
        self.nc = nc
        self.n = 0

    def at(self, name, shape, dtype, off):
        self.n += 1
        return self.nc.alloc_sbuf_tensor_at("%s_%d" % (name, self.n), list(shape), dtype, offset=off)


def nbytes(shape, dtype):
    n = 1
    for s in shape[1:]:
        n *= s
    return n * (4 if dtype == F32 else 2)


class Arena:
    def __init__(self, sb, base, limit):
        self.sb, self.off, self.limit = sb, base, limit

    def alloc(self, name, shape, dtype):
        sz = (nbytes(shape, dtype) + 31) // 32 * 32
        t = self.sb.at(name, shape, dtype, self.off)
        self.off += sz
        if self.off > self.limit:
            raise RuntimeError("SBUF arena overflow at %s: %d > %d" % (name, self.off, self.limit))
        return t


def mm(p, ps, ps_keys, lhsT, rhs, rkeys, start, stop):
    p.op("pe", lambda e: e.matmul(ps, lhsT, rhs, start=start, stop=stop, skip_group_check=True), reads=rkeys,
         writes=ps_keys)


def rmsnorm_tile(p, xt, xkey, g_ap, hT, hkey, sq, sqkeys, ones_bf, ps_sum, ps_key, rstd, rstd_key, nelem, ckey="consts", sq_eng="pool"):
    for c in range(KC):
        s = sq[c % 2]
        sk = sqkeys[c % 2]
        if sq_eng == "act":
            p.op("act", lambda e, s=s, c=c: e.activation(s[:, :], xt[:, c, :], AF.Square), reads=[xkey], writes=[sk])
        else:
            p.op("pool", lambda e, s=s, c=c: e.tensor_tensor(s[:, :], xt[:, c, :], xt[:, c, :], ALU.mult),
                 reads=[xkey], writes=[sk])
        mm(p, ps_sum[:, :], [ps_key], ones_bf, s[:, :], [sk, ckey], c == 0, c == KC - 1)
    p.op("act", lambda e: e.activation(rstd[:, :], ps_sum[:, :], AF.Ln, bias=EPS, scale=1.0 / nelem),
         reads=[ps_key], writes=[rstd_key])
    p.op("act", lambda e: e.activation(rstd[:, :], rstd[:, :], AF.Exp, scale=-0.5),
         reads=[rstd_key], writes=[rstd_key])
    for c in range(KC):
        p.op("dve", lambda e, c=c: e.scalar_tensor_tensor(hT[:, c, :], xt[:, c, :], g_ap[:, c:c + 1], rstd[:, :],
                                                            ALU.mult, ALU.mult),
             reads=[xkey, rstd_key, ckey], writes=[hkey])


class Views:
    def __init__(self, ar, name, dtype, *shapes):
        off = ar.off
        self.v = [ar.sb.at(name, shp, dtype, off) for shp in shapes]
        sz = (nbytes(shapes[0], dtype) + 31) // 32 * 32
        ar.off += sz
        if ar.off > ar.limit:
            raise RuntimeError("SBUF arena overflow at %s" % name)


WBYTES = (2 * KC * DFF + FC * D) * 2


def ffn_walloc(sb):
    wg = sb.at("wg", [128, KC, DFF], BF16, SBASE)
    wu = sb.at("wu", [128, KC, DFF], BF16, SBASE + KC * DFF * 2)
    wd = sb.at("wd", [128, FC, D], BF16, SBASE + 2 * KC * DFF * 2)
    return wg, wu, wd


WCH = 4
FCH = [(0, 2), (2, 6), (6, 14), (14, 22)]


def wkey(tag, nm, f):
    for gi, (f0, f1) in enumerate(FCH):
        if f0 <= f < f1:
            return "%s%s%d" % (tag, nm, gi)
    raise ValueError


def load_ffn_weights(p, tag, W, wg_d, wu_d, wd_d, bg=False):
    wg, wu, wd = W
    wgv = wg_d.rearrange("(k p) f -> p k f", p=128)
    wuv = wu_d.rearrange("(k p) f -> p k f", p=128)
    wdv = wd_d.rearrange("(f p) d -> p f d", p=128)
    for gi, (f0, f1) in enumerate(FCH):
        c0, c1 = f0 * 128, f1 * 128
        p.op("pool", lambda e, c0=c0, c1=c1: e.dma_start(out=wg[:, :, c0:c1], in_=wgv[:, :, c0:c1]),
             writes=["%swg%d" % (tag, gi)], dma=True, semkey="%swg%d" % (tag, gi), bg=bg)
        p.op("pool", lambda e, c0=c0, c1=c1: e.dma_start(out=wu[:, :, c0:c1], in_=wuv[:, :, c0:c1]),
             writes=["%swu%d" % (tag, gi)], dma=True, semkey="%swu%d" % (tag, gi), bg=bg)
    for f in range(0, FC, 2):
        p.op("pool", lambda e, f=f: e.dma_start(out=wd[:, f:f + 2, :], in_=wdv[:, f:f + 2, :]), writes=[tag + "wd"],
             dma=True, semkey=tag + "wd", bg=bg)


def ffn_phase(p, sb, psb, tag, src, dst, W, gain_d, ones_d, ntiles, after_consts=None):
    wg, wu, wd = W
    ar = Arena(sb, SBASE + WBYTES, SLIMIT)
    xt = [ar.alloc("x", [128, KC, TT], F32) for _ in range(2)]
    hT = ar.alloc("hT", [128, KC, TT], BF16)
    act = ar.alloc("act", [128, FC, TT], BF16)
    rstd = ar.alloc("rstd", [128, TT], F32)
    sg = [ar.alloc("sg", [128, TT], F32) for _ in range(2)]
    sq = [ar.alloc("sq", [128, TT], BF16) for _ in range(2)]
    ones_bf = ar.alloc("ones", [128, 128], BF16)
    gain = ar.alloc("gain", [128, KC], F32)

    K = lambda *a: (tag,) + a
    p.op("pool", lambda e: e.dma_start(out=ones_bf[:, :], in_=ones_d), writes=["consts"], dma=True,
         semkey=tag + "c")
    p.op("sp", lambda e: e.dma_start(out=gain[:, :], in_=gain_d), writes=["consts"], dma=True, semkey=tag + "c")
    if after_consts is not None:
        after_consts()
    xsrc = src.rearrange("(c p) n -> p c n", p=128)
    xdst = dst.rearrange("(c p) n -> p c n", p=128)

    def load_x(i):
        s = i % 2
        p.op("sp", lambda e: e.dma_start(out=xt[s][:, :, :], in_=xsrc[:, :, i * TT:(i + 1) * TT]),
             writes=[K("x", s)], dma=True, semkey=tag + "x%d" % s)

    def norm(i):
        rmsnorm_tile(p, xt[i % 2], K("x", i % 2), gain, hT, K("hT"), sq, [K("sq", 0), K("sq", 1)], ones_bf[:, :],
                     psb[7], ("ps", 7), rstd, K("rstd"), float(D), sq_eng="act")

    load_x(0)
    norm(0)
    for i in range(ntiles):
        s = i % 2
        if i + 1 < ntiles:
            load_x(i + 1)
        x = xt[s]
        for f in range(FC):
            gb, ub = psb[(f % 2) * 2], psb[(f % 2) * 2 + 1]
            gk, uk = ("ps", (f % 2) * 2), ("ps", (f % 2) * 2 + 1)
            for k in range(KC):
                mm(p, gb[:, :], [gk], wg[:, k, f * 128:(f + 1) * 128], hT[:, k, :], [K("hT"), wkey(tag, "wg", f)],
                   k == 0, k == KC - 1)
            for k in range(KC):
                mm(p, ub[:, :], [uk], wu[:, k, f * 128:(f + 1) * 128], hT[:, k, :], [K("hT"), wkey(tag, "wu", f)],
                   k == 0, k == KC - 1)
            sgt = sg[f % 2]
            p.op("act", lambda e, sgt=sgt, gb=gb: e.activation(sgt[:, :], gb[:, :], AF.Silu),
                 reads=[gk], writes=[K("sg", f % 2)])
            p.op("dve", lambda e, sgt=sgt, ub=ub, f=f: e.tensor_tensor(act[:, f, :], sgt[:, :], ub[:, :], ALU.mult),
                 reads=[K("sg", f % 2), uk], writes=[K("act", f)])
        if i + 1 < ntiles:
            norm(i + 1)
        for j in range(KC):
            yb = psb[4 + (j % 2)]
            yk = ("ps", 4 + (j % 2))
            for f in range(FC):
                mm(p, yb[:, :], [yk], wd[:, f, j * 128:(j + 1) * 128], act[:, f, :], [K("act", f), tag + "wd"],
                   f == 0, f == FC - 1)
            p.op("dve", lambda e, yb=yb, j=j, x=x: e.scalar_tensor_tensor(x[:, j, :], yb[:, :], 0.5, x[:, j, :],
                                                                          ALU.mult, ALU.add),
                 reads=[yk, K("x", s)], writes=[K("x", s)])
        p.op("act", lambda e, x=x, i=i: e.dma_start(out=xdst[:, :, i * TT:(i + 1) * TT], in_=x[:, :, :]),
             reads=[K("x", s)], writes=[K("xo", s)], dma=True, semkey=tag + "o%d" % s)


C_ONES, C_BLK64, C_NTRI8, C_NONES8, C_MSB, C_BTRI2, C_BLKU, C_ZERO, NCONST = 0, 128, 256, 384, 512, 640, 1152, 1280, 1408


def mix_phase(p, sb, psb, psb6, x1T, sbqT, sbkT, sbv_d, mixedT, w, ntiles, stop=99):
    tag = "m"
    K = lambda *a: (tag,) + a
    ar = Arena(sb, SBASE, SLIMIT)
    win = ar.alloc("win", [128, KC, DIN], BF16)
    wup = ar.alloc("wup", [32, 256], BF16)
    call = ar.alloc("call", [128, NCONST], BF16)
    cvec = ar.alloc("cvec", [128, 4], F32)
    gain = ar.alloc("gain", [128, KC], F32)
    xt = [ar.alloc("x", [128, KC, TT], F32) for _ in range(2)]
    hTs = [ar.alloc("hT", [128, KC, TT], BF16) for _ in range(2)]
    curh = {}
    sq = [ar.alloc("sq", [128, TT], BF16) for _ in range(2)]
    rstd = ar.alloc("rstd", [128, TT], F32)
    qg4 = ar.alloc("qg", [128, 2, 4, 128], F32)
    kg4 = ar.alloc("kg", [128, 2, 4, 128], F32)
    flat = lambda ap: ap.rearrange("p a b -> p (a b)")
    sgate = ar.alloc("sgate", [128, 4, TT], F32)
    lrT = ar.alloc("lrT", [32, TT], BF16)
    hsq = [ar.alloc("hsq", [128, TT], BF16) for _ in range(2)]
    hrstd = [ar.alloc("hrstd", [128, TT], F32) for _ in range(2)]
    sbqk = ar.alloc("sbqk", [128, 8, TT], BF16)
    ktok = ar.alloc("ktok", [128, 4, 256], F32)
    vtok = ar.alloc("vtok", [128, 4, 512], BF16)
    sbv = ar.alloc("sbv", [128, 4, 4, 128], BF16)
    esp = ar.alloc("esp", [128, 256], F32)
    sptok = ar.alloc("sptok", [128, 4, 256], BF16)
    qd = ar.alloc("qd", [128, 2, 4, 128], F32)
    kd = ar.alloc("kd", [128, 4, 128], F32)
    qdecP = [ar.alloc("qdecP", [128, 2, 4, 128], BF16) for _ in range(2)]
    kinv = ar.alloc("kinv", [128, 2, 4, 128], BF16)
    er = ar.alloc("er", [128, 256], F32)
    kendP = [[ar.alloc("kendP", [128, 4, 2, 2, 64], BF16) for _ in range(2)] for _ in range(2)]
    scm = [ar.alloc("scm", [128, 512], BF16) for _ in range(2)]
    S32 = ar.alloc("S32", [128, 2, 128], F32)
    Sbf = [ar.alloc("Sbf", [128, 2, 128], BF16) for _ in range(2)]
    o32_4 = ar.alloc("o32", [128, 4, 4, 128], F32)
    osq = [ar.alloc("osq", [128, TT], BF16) for _ in range(2)]
    orstd = ar.alloc("orstd", [128, TT], F32)
    omix = ar.alloc("omix", [128, 4, TT], BF16)

    p.op("pool", lambda e: e.dma_start(out=call[:, :], in_=w["c_all"]), writes=["mconsts"], dma=True, semkey="mc")
    p.op("sp", lambda e: e.dma_start(out=cvec[:, :], in_=w["c_vec"]), writes=["mconsts"], dma=True, semkey="mc")
    p.op("sp", lambda e: e.dma_start(out=gain[:, :], in_=w["mix_norm"]), writes=["mconsts"], dma=True, semkey="mc")
    p.op("pool", lambda e: e.dma_start(out=wup[0:16, :], in_=w["w_gk_up"]), writes=["wup"], dma=True, semkey="mc")
    p.op("pool", lambda e: e.dma_start(out=wup[16:17, :], in_=w["b_gk"]), writes=["wup"], dma=True, semkey="mc")
    winv = w["w_in"].rearrange("(k p) f -> p k f", p=128)
    for k in range(KC):
        p.op("pool", lambda e, k=k: e.dma_start(out=win[:, k, :], in_=winv[:, k, :]), writes=["win"], dma=True,
             semkey="mw")
    p.op("pool", lambda e: e.memset(lrT[:, :], 1.0), writes=[K("lrT")])
    for hh in range(2):
        p.op("pool", lambda e, hh=hh: e.memset(qdecP[hh][:, :, :, :], 0.0), writes=[K("qdec", 0), K("qdec", 1)])
        for half in range(2):
            p.op("pool", lambda e, hh=hh, half=half: e.memset(kendP[half][hh][:, :, :, :, :], 0.0),
                 writes=[K("kend", tb) for tb in range(4)])
    ones_bf = call[:, C_ONES:C_ONES + 128]
    blk64 = call[:, C_BLK64:C_BLK64 + 128]
    btri4 = call[:, C_BTRI2:C_BTRI2 + 512]
    btri = call[:, C_BTRI2:C_BTRI2 + 128]
    blku = call[:, C_BLKU:C_BLKU + 128]

    xsrc = x1T.rearrange("(c p) n -> p c n", p=128)
    sbq_v = sbqT.rearrange("(c p) n -> p c n", p=128)
    sbk_v = sbkT.rearrange("(c p) n -> p c n", p=128)
    sbv_v = sbv_d.rearrange("c p n f -> p c n f")
    mix_v = mixedT.rearrange("(c p) n -> p c n", p=128)

    def load_x(i):
        s = i % 2
        p.op("sp", lambda e: e.dma_start(out=xt[s][:, :, :], in_=xsrc[:, :, i * TT:(i + 1) * TT]),
             writes=[K("x", s)], dma=True, semkey="mx%d" % s)

    fmb = [0]

    def proj_fm(col, M):
        b = fmb[0] % 2
        fmb[0] += 1
        bank, bkey = psb[b], ("ps", b)
        for k in range(KC):
            mm(p, bank[0:M, :], [bkey], win[:, k, col:col + M], curh["hT"][:, k, :], [curh["hk"], "win"],
               k == 0, k == KC - 1)
        return bank, bkey

    tmb = [0]

    def proj_tm(tb, col, N):
        b = 3 + tmb[0] % 2
        tmb[0] += 1
        bank, bkey = psb[b], ("ps", b)
        for k in range(KC):
            mm(p, bank[:, 0:N], [bkey], curh["hT"][:, k, tb * 128:(tb + 1) * 128], win[:, k, col:col + N],
               [curh["hk"], "win"], k == 0, k == KC - 1)
        return bank, bkey

    def norm(i):
        rmsnorm_tile(p, xt[i % 2], K("x", i % 2), gain, hTs[i % 2], K("hT", i % 2), sq, [K("sq", 0), K("sq", 1)],
                     ones_bf, psb[2], ("ps", 2), rstd, K("rstd"), float(D), ckey="mconsts", sq_eng="act")

    load_x(0)
    norm(0)
    nsc = 0
    chunk_ctr = 0
    for i in range(ntiles):
        s = i % 2
        cols = slice(i * TT, (i + 1) * TT)
        if i + 1 < ntiles:
            load_x(i + 1)
        curh["hT"], curh["hk"] = hTs[i % 2], K("hT", i % 2)
        bank, bkey = proj_fm(1536, 16)
        p.op("act", lambda e, bank=bank: e.copy(lrT[0:16, :], bank[0:16, :]), reads=[bkey], writes=[K("lrT")])
        for pr in range(2):
            bank, bkey = proj_fm(pr * 128, 128)
            p.op("act", lambda e, bank=bank, pr=pr: e.copy(flat(qg4[:, pr, :, :]), bank[:, :]), reads=[bkey], writes=[K("qg")])
            bank, bkey = proj_fm(256 + pr * 128, 128)
            p.op("dve", lambda e, bank=bank, pr=pr: e.tensor_copy(flat(kg4[:, pr, :, :]), bank[:, :]), reads=[bkey],
                 writes=[K("kg")])
        if stop <= 1:
            continue
        for tb in range(4):
            bank, bkey = proj_tm(tb, 256, 512)
            p.op("dve", lambda e, bank=bank, tb=tb: e.tensor_copy(ktok[:, tb, :], bank[:, 0:256]), reads=[bkey],
                 writes=[K("ktok", tb)])
            p.op("dve", lambda e, bank=bank, tb=tb: e.tensor_copy(vtok[:, tb, 0:256], bank[:, 256:512]),
                 reads=[bkey], writes=[K("vtok", tb)])
            bank, bkey = proj_tm(tb, 768, 256)
            p.op("act", lambda e, bank=bank, tb=tb: e.copy(vtok[:, tb, 256:512], bank[:, 0:256]),
                 reads=[bkey], writes=[K("vtok", tb)])
            bank, bkey = proj_tm(tb, 2576, 512)
            p.op("act", lambda e, bank=bank, tb=tb: e.copy(sbv[:, :, tb, :],
                                                           bank[:, :].rearrange("p (c f) -> p c f", c=4)),
                 reads=[bkey], writes=[K("sbv")])
        p.op("sp", lambda e, i=i: e.dma_start(out=sbv_v[:, :, i * 4:(i + 1) * 4, :], in_=sbv[:, :, :, :]),
             reads=[K("sbv")], writes=[K("sbv_o")], dma=True, semkey="mosbv")
        if i + 1 < ntiles:
            norm(i + 1)
        if stop <= 2:
            continue
        for tb in range(4):
            la = psb[5]
            mm(p, la[:, 0:256], [("ps", 5)], lrT[0:17, tb * 128:(tb + 1) * 128], wup[0:17, :],
               [K("lrT"), "wup"], True, True)
            p.op("act", lambda e, la=la: e.activation(esp[:, :], la[:, 0:256], AF.Exp, scale=-1.0),
                 reads=[("ps", 5)], writes=[K("esp")])
            p.op("act", lambda e, tb=tb: e.activation(sptok[:, tb, :], esp[:, :], AF.Ln, bias=1.0),
                 reads=[K("esp")], writes=[K("sptok", tb)])
            mm(p, la[:, 256:512], [("ps", 5)], blku, sptok[:, tb, :], [K("sptok", tb), "mconsts"], True, True)
            p.op("act", lambda e, la=la: e.activation(er[:, :], la[:, 256:512], AF.Exp, scale=-1.0 / 16),
                 reads=[("ps", 5)], writes=[K("er")])
            for half in range(2):
                trow = slice(half * 64, (half + 1) * 64)
                for hh in range(2):
                    p.op("dve", lambda e, tb=tb, half=half, hh=hh, trow=trow: e.tensor_tensor(
                        kendP[half][hh][trow, tb, :, hh, :],
                        ktok[trow, tb, :].rearrange("p (a b c) -> p a b c", a=2, b=2)[:, :, hh, :],
                        er[trow, :].rearrange("p (a b c) -> p a b c", a=2, b=2)[:, :, hh, :], ALU.mult),
                        reads=[K("ktok", tb), K("er")], writes=[K("kend", tb)])
        for pr in range(2):
            for tb in range(4):
                mm(p, psb6[:, tb, :], [("ps", 6)], sptok[:, tb, pr * 128:(pr + 1) * 128], btri,
                   [K("sptok", tb), "mconsts"], True, True)
            p.op("act", lambda e, pr=pr: e.activation(qd[:, pr, :, :], psb6[:, :, :], AF.Exp, scale=-1.0 / 16),
                 reads=[("ps", 6)], writes=[K("qd", pr)])
            p.op("act", lambda e: e.activation(kd[:, :, :], psb6[:, :, :], AF.Exp, scale=1.0 / 16),
                 reads=[("ps", 6)], writes=[K("kd")])
            for hh in range(2):
                rows = slice(hh * 64, (hh + 1) * 64)
                p.op("dve", lambda e, pr=pr, hh=hh, rows=rows: e.scalar_tensor_tensor(
                    qdecP[hh][rows, pr, :, :], qg4[rows, pr, :, :], 0.125, qd[rows, pr, :, :], ALU.mult, ALU.mult),
                    reads=[K("qg"), K("qd", pr)], writes=[K("qdec", pr)])
            p.op("dve", lambda e, pr=pr: e.tensor_tensor(kinv[:, pr, :, :], kg4[:, pr, :, :], kd[:, :, :], ALU.mult),
                 reads=[K("kg"), K("kd")], writes=[K("kinv", pr)])
        if stop <= 3:
            continue
        for h in range(4):
            bank, bkey = proj_fm(1024 + h * 128, 128)
            p.op("act", lambda e, bank=bank, h=h: e.activation(sgate[:, h, :], bank[:, :], AF.Silu), reads=[bkey],
                 writes=[K("sgate", h)])
        if stop <= 4:
            continue
        if i % (SEQ // TT) == 0:
            p.op("pool", lambda e: e.memset(S32[:, :, :], 0.0), writes=[K("S32")])
            p.op("pool", lambda e, cc=chunk_ctr: e.memset(Sbf[cc % 2][:, :, :], 0.0), writes=[K("Sbf", chunk_ctr % 2)])
        for tb in range(4):
            scb = scm[nsc % 2]
            sck = K("scm", nsc % 2)
            nsc += 1
            for pr in range(2):
                for hh in range(2):
                    h = 2 * pr + hh
                    mm(p, psb[7][:, h * 128:(h + 1) * 128], [("ps", 7)], kinv[:, pr, tb, :],
                       qdecP[hh][:, pr, tb, :], [K("kinv", pr), K("qdec", pr)], True, True)
            p.op("dve", lambda e, scb=scb: e.tensor_tensor(scb[:, :], psb[7][:, :], btri4, ALU.mult),
                 reads=[("ps", 7), "mconsts"], writes=[sck])
            for h in range(4):
                mm(p, psb6[:, h, :], [("ps", 6)], vtok[:, tb, h * 128:(h + 1) * 128],
                   scb[:, h * 128:(h + 1) * 128], [K("vtok", tb), sck], h == 0, False)
            for half in range(2):
                for pr in range(2):
                    reg = (half * 2 + pr) * 128
                    for hh in range(2):
                        h = 2 * pr + hh
                        mm(p, psb[5][:, reg:reg + 128], [("ps", 5)],
                           kendP[half][hh][:, tb, pr, :, :].rearrange("p a b -> p (a b)"),
                           vtok[:, tb, h * 128:(h + 1) * 128], [K("kend", tb), K("vtok", tb)], hh == 0, hh == 1)
            for half in range(2):
                n = chunk_ctr + tb * 2 + half
                cur, nxt = n % 2, (n + 1) % 2
                for pr in range(2):
                    for hh in range(2):
                        h = 2 * pr + hh
                        mm(p, psb6[:, h, half * 64:(half + 1) * 64], [("ps", 6)], Sbf[cur][:, pr, :],
                           qdecP[hh][:, pr, tb, half * 64:(half + 1) * 64], [K("Sbf", cur), K("qdec", pr)],
                           False, half == 1)
                for pr in range(2):
                    reg = (half * 2 + pr) * 128
                    p.op("dve", lambda e, pr=pr, tb=tb, half=half, reg=reg:
                         e.scalar_tensor_tensor(S32[:, pr, :], S32[:, pr, :],
                                                qd[:, pr, tb, half * 64 + 63:half * 64 + 64],
                                                psb[5][:, reg:reg + 128], ALU.mult, ALU.add),
                         reads=[K("S32"), K("qd", pr), ("ps", 5)], writes=[K("S32")])
                p.op("act", lambda e, nxt=nxt: e.copy(Sbf[nxt][:, :, :], S32[:, :, :]),
                     reads=[K("S32")], writes=[K("Sbf", nxt)])
            p.op("act", lambda e, tb=tb: e.copy(o32_4[:, :, tb, :], psb6[:, :, :]), reads=[("ps", 6)],
                 writes=[K("o32")])
        chunk_ctr += 8
        if stop <= 5:
            continue
        for h in range(4):
            oq = osq[h % 2]
            p.op("act", lambda e, oq=oq, h=h: e.activation(oq[:, :], flat(o32_4[:, h, :, :]), AF.Square),
                 reads=[K("o32")], writes=[K("osq", h % 2)])
            mm(p, psb[2][:, :], [("ps", 2)], ones_bf, oq[:, :], [K("osq", h % 2), "mconsts"], True, True)
            p.op("act", lambda e: e.activation(orstd[:, :], psb[2][:, :], AF.Ln, bias=EPS, scale=1.0 / 128),
                 reads=[("ps", 2)], writes=[K("orstd")])
            p.op("act", lambda e: e.activation(orstd[:, :], orstd[:, :], AF.Exp, scale=-0.5),
                 reads=[K("orstd")], writes=[K("orstd")])
            p.op("dve", lambda e, h=h: e.scalar_tensor_tensor(flat(o32_4[:, h, :, :]), flat(o32_4[:, h, :, :]), cvec[:, 2:3],
                                                               orstd[:, :], ALU.mult, ALU.mult),
                 reads=[K("o32"), K("orstd"), "mconsts"], writes=[K("o32")])
            p.op("dve", lambda e, h=h: e.tensor_tensor(omix[:, h, :], flat(o32_4[:, h, :, :]), sgate[:, h, :], ALU.mult),
                 reads=[K("o32"), K("sgate", h)], writes=[K("omix")])
        p.op("sp", lambda e, cols=cols: e.dma_start(out=mix_v[:, 0:4, cols], in_=omix[:, :, :]),
             reads=[K("omix")], writes=[K("omix_o")], dma=True, semkey="moomix")
        if stop <= 6:
            continue
        for idx in range(8):
            col = (1552 if idx < 4 else 2064) + (idx % 4) * 128
            bank, bkey = proj_fm(col, 128)
            hq, hr = hsq[idx % 2], hrstd[idx % 2]
            p.op("act", lambda e, hq=hq, bank=bank: e.activation(hq[:, :], bank[:, :], AF.Square), reads=[bkey],
                 writes=[K("hsq", idx % 2)])
            mm(p, psb[2][:, :], [("ps", 2)], blk64, hq[:, :], [K("hsq", idx % 2), "mconsts"], True, True)
            p.op("act", lambda e, hr=hr: e.activation(hr[:, :], psb[2][:, :], AF.Ln, bias=EPS, scale=1.0 / 64),
                 reads=[("ps", 2)], writes=[K("hrstd", idx % 2)])
            p.op("act", lambda e, hr=hr: e.activation(hr[:, :], hr[:, :], AF.Exp, scale=-0.5),
                 reads=[K("hrstd", idx % 2)], writes=[K("hrstd", idx % 2)])
            gcol = 0 if idx < 4 else 1
            p.op("dve", lambda e, idx=idx, bank=bank, hr=hr, gcol=gcol:
                 e.scalar_tensor_tensor(sbqk[:, idx, :], bank[:, :], cvec[:, gcol:gcol + 1], hr[:, :],
                                        ALU.mult, ALU.mult),
                 reads=[bkey, K("hrstd", idx % 2), "mconsts"], writes=[K("sbqk")])
        p.op("sp", lambda e, cols=cols: e.dma_start(out=sbq_v[:, :, cols], in_=sbqk[:, 0:4, :]),
             reads=[K("sbqk")], writes=[K("sbqk_o")], dma=True, semkey="moqk")
        p.op("sp", lambda e, cols=cols: e.dma_start(out=sbk_v[:, :, cols], in_=sbqk[:, 4:8, :]),
             reads=[K("sbqk")], writes=[K("sbqk_o")], dma=True, semkey="moqk")


def sb_phase(p, sb, psb, sbqT, sbkT, sbv_d, mixedT, w, nseq, nsblk=8, npairs=4, after_consts=None):
    tag = "s"
    K = lambda *a: (tag,) + a
    ar = Arena(sb, SBASE + WBYTES, SLIMIT)
    call = ar.alloc("call", [128, NCONST], BF16)
    qT = [ar.alloc("qT", [128, SEQ], BF16) for _ in range(2)]
    kT = [ar.alloc("kT", [128, SEQ], BF16) for _ in range(2)]
    vv1 = ar.alloc("vv", [128, SEQ // 128, 128], BF16)
    vv = [vv1, vv1]
    ets = [[ar.alloc("et", [128, TT], F32) for _ in range(2)] for _ in range(2)]
    St = [[ar.alloc("St", [128, TT], BF16) for _ in range(2)] for _ in range(3)]
    Ssum = [[[ar.alloc("Ssum", [128, TT], BF16) for _ in range(2)] for _ in range(2)] for _ in range(2)]
    wt = [[ar.alloc("wt", [128, TT], BF16) for _ in range(2)] for _ in range(3)]
    ob = [ar.alloc("ob", [128, TT], BF16) for _ in range(2)]
    p.op("pool", lambda e: e.dma_start(out=call[:, :], in_=w["c_all"]), writes=["sconsts"], dma=True, semkey="sc")
    if after_consts is not None:
        after_consts()
    ntri8 = call[:, C_NTRI8:C_NTRI8 + 128]
    nones8 = call[:, C_NONES8:C_NONES8 + 128]
    msb = call[:, C_MSB:C_MSB + 128]
    zero = call[:, C_ZERO:C_ZERO + 128]
    obanks = [psb[6][:, :, :].rearrange("p a b -> p (a b)"), psb[7][:, :]]
    okeys = [("ps", 6), ("ps", 7)]
    tasks = []
    it = 0
    nsb = 0
    for b in range(nseq):
        for c in range(npairs):
            s = it % 2
            it += 1
            for i in range(nsblk):
                nb = 4 * i + 4
                for t in range(nb):
                    tasks.append(dict(b=b, c=c, s=s, i=i, t=t, kb=4 * i + 3 - t, nb=nb, nsb=nsb,
                                      load=(i == 0 and t == 0)))
                nsb += 1
    G = len(tasks)

    def load_vv(b, c):
        p.op("sp", lambda e: e.dma_start(out=vv1[:, :, :], in_=sbv_d[c, :, b * 32:(b + 1) * 32, :]),
             writes=[K("vv")], dma=True, semkey="sv")

    def bank(g, hh):
        k = (g % 3) * 2 + hh
        return psb[k], ("ps", k)

    def stage1(g):
        T = tasks[g]
        b, c, s, i, t, kb = T["b"], T["c"], T["s"], T["i"], T["t"], T["kb"]
        par = T["nsb"] % 2
        if T["load"]:
            tcols = slice(b * SEQ, (b + 1) * SEQ)
            p.op("sp", lambda e: e.dma_start(out=qT[s][:, :], in_=sbqT[c * 128:(c + 1) * 128, tcols]),
                 writes=[K("qT", s)], dma=True, semkey="sq%d" % s)
            p.op("sp", lambda e: e.dma_start(out=kT[s][:, :], in_=sbkT[c * 128:(c + 1) * 128, tcols]),
                 writes=[K("kT", s)], dma=True, semkey="sq%d" % s)
            if g == 0:
                load_vv(b, c)
        if t == 0:
            mm(p, obanks[par], [okeys[par]], zero, qT[s][:, 0:512], ["sconsts", K("qT", s)], True, False)
            for hh in range(2):
                for j in range(2):
                    p.op("dve", lambda e, hh=hh, j=j: e.memset(Ssum[par][hh][j][:, :], 0.0),
                         writes=[K("Ssum", par, hh, j)])
        r = kb - 4 * i
        c0 = 128 * max(r, 0)
        T["c0"], T["diag"] = c0, r >= 0
        q0, q1 = i * 512 + c0, (i + 1) * 512
        for hh in range(2):
            rows = slice(hh * 64, (hh + 1) * 64)
            A, Ak = bank(g, hh)
            mm(p, A[:, c0:512], [Ak], kT[s][rows, kb * 128:(kb + 1) * 128], qT[s][rows, q0:q1],
               [K("kT", s), K("qT", s)], True, False)
        for hh in range(2):
            A, Ak = bank(g, hh)
            et = ets[g % 2][hh]
            p.op("act", lambda e, et=et, A=A: e.activation(et[:, c0:512], A[:, c0:512], AF.Exp, scale=0.125),
                 reads=[Ak], writes=[K("et", g % 2, hh)])

    def stage1b(g):
        T = tasks[g]
        t, c0, par = T["t"], T["c0"], T["nsb"] % 2
        for hh in range(2):
            et = ets[g % 2][hh]
            S_t, Sk = St[g % 3][hh], K("St", g % 3, hh)
            p.op("act", lambda e, et=et, S_t=S_t: e.activation(S_t[:, c0:512], et[:, c0:512], AF.Ln, bias=1.0),
                 reads=[K("et", g % 2, hh)], writes=[Sk])
            if T["diag"]:
                p.op("dve", lambda e, S_t=S_t: e.tensor_tensor(S_t[:, c0:c0 + 128], S_t[:, c0:c0 + 128], msb,
                                                               ALU.mult),
                     reads=[Sk, "sconsts"], writes=[Sk])
            if t + 1 < T["nb"]:
                src, dst = Ssum[par][hh][t % 2], Ssum[par][hh][(t + 1) % 2]
                p.op("dve", lambda e, src=src, dst=dst, S_t=S_t: e.tensor_tensor(dst[:, c0:512], src[:, c0:512],
                                                                                 S_t[:, c0:512], ALU.add),
                     reads=[Sk, K("Ssum", par, hh, t % 2)], writes=[K("Ssum", par, hh, (t + 1) % 2)])

    def stage2(g):
        T = tasks[g]
        t, c0, par = T["t"], T["c0"], T["nsb"] % 2
        for hh in range(2):
            A, Ak = bank(g, hh)
            S_t, Sk = St[g % 3][hh], K("St", g % 3, hh)
            mm(p, A[:, c0:512], [Ak], ntri8, S_t[:, c0:512], [Sk, "sconsts"], False, t == 0)
            if t > 0:
                mm(p, A[:, c0:512], [Ak], nones8, Ssum[par][hh][t % 2][:, c0:512],
                   [K("Ssum", par, hh, t % 2), "sconsts"], False, True)
        for hh in range(2):
            A, Ak = bank(g, hh)
            w_t, wk = wt[g % 3][hh], K("wt", g % 3, hh)
            p.op("act", lambda e, w_t=w_t, A=A: e.activation(w_t[:, c0:512], A[:, c0:512], AF.Exp, scale=0.125),
                 reads=[Ak], writes=[wk])
            if T["diag"]:
                p.op("dve", lambda e, w_t=w_t: e.tensor_tensor(w_t[:, c0:c0 + 128], w_t[:, c0:c0 + 128], msb,
                                                               ALU.mult),
                     reads=[wk, "sconsts"], writes=[wk])

    def stage3(g):
        T = tasks[g]
        b, c, s, i, t, kb, c0 = T["b"], T["c"], T["s"], T["i"], T["t"], T["kb"], T["c0"]
        par = T["nsb"] % 2
        obank, okey = obanks[par], okeys[par]
        for hh in range(2):
            w_t, wk = wt[g % 3][hh], K("wt", g % 3, hh)
            lhsT = vv[s][:, kb, hh * 64:(hh + 1) * 64]
            out = obank[hh * 64:(hh + 1) * 64, c0:512]
            rhs = w_t[:, c0:512]
            p.op("pe", lambda e, out=out, lhsT=lhsT, rhs=rhs, hh=hh: e.matmul(
                out, lhsT, rhs, start=False, stop=(kb == 0), skip_group_check=True, tile_position=(0, hh * 64)),
                reads=[K("vv"), wk], writes=[okey])
        if kb == 0:
            o_s, osk = ob[par], K("ob", par)
            p.op("dve", lambda e: e.tensor_copy(o_s[:, :], obank), reads=[okey], writes=[osk])
            tok0 = b * SEQ + i * 512
            p.op("sp", lambda e: e.dma_start(out=mixedT[512 + c * 128:512 + (c + 1) * 128, tok0:tok0 + 512],
                                             in_=o_s[:, :]),
                 reads=[osk], writes=[osk + ("o",)], dma=True, semkey="so%d" % par)

    for step in range(G + 2):
        if step < G:
            stage1(step)
        if 0 <= step - 1 < G:
            stage2(step - 1)
        if step < G:
            stage1b(step)
        if 0 <= step - 2 < G:
            stage3(step - 2)
            g3 = step - 2
            if g3 + 1 < G and tasks[g3 + 1]["load"]:
                load_vv(tasks[g3 + 1]["b"], tasks[g3 + 1]["c"])


def wout_phase(p, sb, psb, x1T, mixedT, w, ntiles):
    tag = "o"
    K = lambda *a: (tag,) + a
    ar = Arena(sb, SBASE + WBYTES, SLIMIT)
    wout = ar.alloc("wout", [128, KC, D], BF16)
    xt = [ar.alloc("x", [128, KC, TT], F32) for _ in range(2)]
    mt = [ar.alloc("mt", [128, KC, TT], BF16) for _ in range(2)]
    wv = w["w_out"].rearrange("(k p) f -> p k f", p=128)
    for k in range(0, KC, 2):
        p.op("pool", lambda e, k=k: e.dma_start(out=wout[:, k:k + 2, :], in_=wv[:, k:k + 2, :]), writes=["wout"],
             dma=True, semkey="ow")
    xv = x1T.rearrange("(c p) n -> p c n", p=128)
    mv = mixedT.rearrange("(c p) n -> p c n", p=128)

    def load(i):
        s = i % 2
        cols = slice(i * TT, (i + 1) * TT)
        p.op("sp", lambda e: e.dma_start(out=xt[s][:, :, :], in_=xv[:, :, cols]), writes=[K("x", s)], dma=True,
             semkey="ox%d" % s)
        p.op("sp", lambda e: e.dma_start(out=mt[s][:, :, :], in_=mv[:, :, cols]), writes=[K("m", s)], dma=True,
             semkey="ox%d" % s)

    load(0)
    for i in range(ntiles):
        s = i % 2
        if i + 1 < ntiles:
            load(i + 1)
        for j in range(KC):
            b = j % 4
            for c in range(KC):
                mm(p, psb[b][:, :], [("ps", b)], wout[:, c, j * 128:(j + 1) * 128], mt[s][:, c, :],
                   [K("m", s), "wout"], c == 0, c == KC - 1)
            p.op("dve", lambda e, b=b, j=j, s=s: e.tensor_tensor(xt[s][:, j, :], xt[s][:, j, :], psb[b][:, :], ALU.add),
                 reads=[("ps", b), K("x", s)], writes=[K("x", s)])
        p.op("act", lambda e, s=s, i=i: e.dma_start(out=xv[:, :, i * TT:(i + 1) * TT], in_=xt[s][:, :, :]),
             reads=[K("x", s)], writes=[K("xo", s)], dma=True, semkey="oo%d" % s)


def build(cfg):
    from contextlib import ExitStack
    nc = bass.Bass("TRN2", target_bir_lowering=False)
    dt = lambda name, shape, dtype=F32, kind="ExternalInput": nc.dram_tensor(name, list(shape), dtype, kind=kind).ap()
    xT = dt("xT", [D, NTOK])
    yT = dt("yT", [D, NTOK], kind="ExternalOutput")
    w = {}
    for nm, shp in (("ffn1_w_gate", [D, DFF]), ("ffn1_w_up", [D, DFF]), ("ffn1_w_down", [DFF, D]),
                    ("ffn2_w_gate", [D, DFF]), ("ffn2_w_up", [D, DFF]), ("ffn2_w_down", [DFF, D]),
                    ("w_in", [D, DIN]), ("w_out", [D, D]), ("w_gk_up", [16, 256]), ("b_gk", [1, 256]),
                    ("ffn1_norm", [128, KC]), ("mix_norm", [128, KC]), ("ffn2_norm", [128, KC]),
                    ("c_vec", [128, 4]), ("c_ones", [128, 128]), ("c_all", [128, NCONST])):
        w[nm] = dt(nm, shp)
    dbg = cfg.get("debug", False)
    skind = "ExternalOutput" if dbg else "Internal"
    x1T = dt("x1T", [D, NTOK], F32, kind=skind)
    sbqT = dt("sbqT", [512, NTOK], BF16, kind=skind)
    sbkT = dt("sbkT", [512, NTOK], BF16, kind=skind)
    sbv_d = dt("sbv", [4, 128, NTOK // 128, 128], BF16, kind=skind)
    mixedT = dt("mixedT", [D, NTOK], BF16, kind=skind)
    sb = SB(nc)
    psb = [nc.alloc_psum_tensor("psb%d" % i, [128, 512], F32) for i in range(6)]
    psb6 = nc.alloc_psum_tensor("psb6", [128, 4, 128], F32)
    psb7 = nc.alloc_psum_tensor("psb7", [128, 512], F32)
    p = Prog(nc)
    ntiles = cfg.get("ntiles", NTOK // TT)
    phases = cfg.get("phases", "1234")
    W = ffn_walloc(sb)

    pb = psb + [psb6, psb7]
    if "1" in phases:
        ffn_phase(p, sb, pb, "f1", xT, x1T if len(phases) > 1 else yT, W, w["ffn1_norm"], w["c_ones"], ntiles,
                  after_consts=lambda: load_ffn_weights(p, "f1", W, w["ffn1_w_gate"], w["ffn1_w_up"],
                                                        w["ffn1_w_down"]))
        p.barrier()
    if "2" in phases:
        mix_phase(p, sb, pb, psb6, x1T, sbqT, sbkT, sbv_d, mixedT, w, ntiles, stop=cfg.get('mixstop', 99))
        p.barrier()
    if "3" in phases:
        sb_phase(p, sb, pb, sbqT, sbkT, sbv_d, mixedT, w, cfg.get("nseq", 2), cfg.get("nsblk", 8),
                 cfg.get("npairs", 4),
                 after_consts=lambda: load_ffn_weights(p, "f2", W, w["ffn2_w_gate"], w["ffn2_w_up"],
                                                       w["ffn2_w_down"], bg=True))
        p.barrier()
    if "4" in phases:
        wout_phase(p, sb, pb, x1T, mixedT, w, ntiles)
        p.barrier()
        ffn_phase(p, sb, pb, "f2", x1T, yT, W, w["ffn2_norm"], w["c_ones"], ntiles)
    with ExitStack() as stack:
        p.emit(stack)
    return nc


def host_consts():
    idx = np.arange(128)
    j, t = idx[:, None], idx[None, :]
    same = (j // 64) == (t // 64)
    call = np.zeros((128, NCONST), np.float32)
    call[:, C_ONES:C_ONES + 128] = 1.0
    call[:, C_BLK64:C_BLK64 + 128] = same
    call[:, C_NTRI8:C_NTRI8 + 128] = np.where(j >= t, -8.0, 0.0)
    call[:, C_NONES8:C_NONES8 + 128] = -8.0
    call[:, C_MSB:C_MSB + 128] = (j < t)
    bt = (same & (j <= t)).astype(np.float32)
    for rep in range(4):
        call[:, C_BTRI2 + rep * 128:C_BTRI2 + (rep + 1) * 128] = bt
    call[:, C_BLKU:C_BLKU + 128] = (same & (j > t))
    return {"c_ones": np.ones((128, 128), np.float32), "c_all": call}


def kernel(**inputs):
    cfg = inputs.pop("_cfg", {})
    x = np.asarray(inputs["x"], dtype=np.float32)
    B, T, Dm = x.shape
    xs = x.reshape(NCORES, NTOK, Dm)
    consts = host_consts()
    shared = {}
    for k, v in inputs.items():
        if k == "x":
            continue
        a = np.ascontiguousarray(np.asarray(v, dtype=np.float32))
        a = a.reshape(a.shape[1:])
        if k == "b_gk":
            a = a.reshape(1, 256)
        if k in ("ffn1_norm", "mix_norm", "ffn2_norm"):
            a = np.ascontiguousarray(a.reshape(KC, 128).T)
        shared[k] = a
    cvec = np.zeros((128, 4), np.float32)
    cvec[:, 0] = np.tile(shared.pop("sb_q_norm").reshape(64), 2)
    cvec[:, 1] = np.tile(shared.pop("sb_k_norm").reshape(64), 2)
    cvec[:, 2] = shared.pop("gla_out_norm").reshape(128)
    shared["c_vec"] = cvec
    shared.update(consts)
    in_maps = []
    for c in range(NCORES):
        m = dict(shared)
        m["xT"] = np.ascontiguousarray(xs[c].T)
        in_maps.append(m)
    nc = build(cfg)
    if cfg.get("trace"):
        res = run_bass_kernel_spmd(nc, in_maps, core_ids=list(range(NCORES)), trace=True)
        print("EXEC_TIME_NS", res.exec_time_ns)
    else:
        res = run_bass_kernel_spmd(nc, in_maps, core_ids=list(range(NCORES)))
    if cfg.get("raw"):
        return res
    out = np.empty((NCORES, NTOK, Dm), np.float32)
    for c in range(NCORES):
        out[c] = res.results[c]["yT"].T
    return out.reshape(B, T, Dm)
```
